# Optimizing a Trainium2 kernel written in Bass

```python
import math
import jax, jax.numpy as jnp
from jax import lax
import numpy as np

D_MODEL = 1024
BATCH = 2
SEQ = 8192
DEPTH = 1

ATT_HEADS = 8
ATT_KV_HEADS = 2
HEAD_DIM = 64
WINDOW = 128
BLOCK = 128
ROPE_THETA = 10000.0
CONV_CH = 512
CONV_WIDTH = 31
MEM_LEN = 256
MEM_HEADS = 4
MEM_HEAD_DIM = 128
N_BRANCHES = 3
N_GROUPS = 4
EXPERTS_PER_GROUP = 4
N_EXPERTS = N_GROUPS * EXPERTS_PER_GROUP
TOP_K_IN_GROUP = 2
EXPERT_FF = 512
EPS = 1e-6
LN_EPS = 1e-5
NEG_INF = -1e30

Q_WIDTH = ATT_HEADS * HEAD_DIM
KV_WIDTH = ATT_KV_HEADS * HEAD_DIM
GLU_WIDTH = 2 * CONV_CH
XQ_WIDTH = MEM_HEADS * MEM_HEAD_DIM
GATE_WIDTH = N_BRANCHES * D_MODEL
IN_WIDTH = Q_WIDTH + 2 * KV_WIDTH + GLU_WIDTH + XQ_WIDTH + GATE_WIDTH

kernel_name = "hybrid_swa_conformer_memxattn_hiermoe"


def rms_norm(x, g):
    xf = x.astype(jnp.float32)
    y = xf * lax.rsqrt(jnp.mean(xf * xf, axis=-1, keepdims=True) + EPS)
    return (y * g.astype(jnp.float32)).astype(x.dtype)


def layer_norm(x, g, b):
    xf = x.astype(jnp.float32)
    mu = jnp.mean(xf, axis=-1, keepdims=True)
    var = jnp.mean(jnp.square(xf - mu), axis=-1, keepdims=True)
    y = (xf - mu) * lax.rsqrt(var + LN_EPS)
    return (y * g.astype(jnp.float32) + b.astype(jnp.float32)).astype(x.dtype)


def rope_tables(positions):
    inv_freq = 1.0 / (ROPE_THETA ** (jnp.arange(0, HEAD_DIM, 2, dtype=jnp.float32) / HEAD_DIM))
    ang = positions.astype(jnp.float32)[..., None] * inv_freq
    return jnp.cos(ang), jnp.sin(ang)


def apply_rope(t, cos, sin):
    tf = t.astype(jnp.float32)
    t1, t2 = jnp.split(tf, 2, axis=-1)
    c = cos[:, :, None, :]
    s = sin[:, :, None, :]
    out = jnp.concatenate([t1 * c - t2 * s, t2 * c + t1 * s], axis=-1)
    return out.astype(t.dtype)


def split_columns(proj, widths):
    points = []
    acc = 0
    for w in widths[:-1]:
        acc += w
        points.append(acc)
    return jnp.split(proj, points, axis=-1)


def sliding_window_attention(q, k, v, sinks):
    B, S, Hq, Dh = q.shape
    G = Hq // ATT_KV_HEADS
    nb = S // BLOCK
    qb = q.reshape(B, nb, BLOCK, ATT_KV_HEADS, G, Dh)

    def band(t):
        tb = t.reshape(B, nb, BLOCK, ATT_KV_HEADS, Dh)
        prev = jnp.pad(tb, ((0, 0), (1, 0), (0, 0), (0, 0), (0, 0)))[:, :nb]
        return jnp.concatenate([prev, tb], axis=2)

    kb, vb = band(k), band(v)
    s = jnp.einsum('bnqhgd,bnkhd->bnhgqk', qb, kb).astype(jnp.float32) * (Dh ** -0.5)
    q_pos = jnp.arange(nb)[:, None] * BLOCK + jnp.arange(BLOCK)[None, :]
    k_pos = (jnp.arange(nb)[:, None] - 1) * BLOCK + jnp.arange(2 * BLOCK)[None, :]
    delta = q_pos[:, :, None] - k_pos[:, None, :]
    valid = (delta >= 0) & (delta < WINDOW) & (k_pos[:, None, :] >= 0)
    s = jnp.where(valid[None, :, None, None], s, NEG_INF)
    sink = jnp.broadcast_to(sinks.astype(jnp.float32).reshape(1, 1, ATT_KV_HEADS, G, 1, 1),
                            s.shape[:-1] + (1,))
    p = jax.nn.softmax(jnp.concatenate([s, sink], axis=-1), axis=-1)[..., :-1]
    o = jnp.einsum('bnhgqk,bnkhd->bnqhgd', p.astype(v.dtype), vb)
    return o.reshape(B, S, Hq * Dh)


def causal_depthwise_conv(u, w, b):
    y = lax.conv_general_dilated(
        u, w[:, None, :].astype(u.dtype), window_strides=(1,),
        padding=[(CONV_WIDTH - 1, 0)], dimension_numbers=('NWC', 'WIO', 'NWC'),
        feature_group_count=CONV_CH)
    return y + b.astype(u.dtype)


def memory_cross_attention(xq, mem_n, w_kv, g_xq, g_xk):
    B, S, _ = xq.shape
    M = mem_n.shape[1]
    mk, mv = jnp.split(mem_n @ w_kv, 2, axis=-1)
    mk = rms_norm(mk.reshape(B, M, MEM_HEADS, MEM_HEAD_DIM), g_xk)
    mv = mv.reshape(B, M, MEM_HEADS, MEM_HEAD_DIM)
    qh = rms_norm(xq.reshape(B, S, MEM_HEADS, MEM_HEAD_DIM), g_xq)
    s = jnp.einsum('bshd,bmhd->bhsm', qh, mk).astype(jnp.float32) * (MEM_HEAD_DIM ** -0.5)
    p = jax.nn.softmax(s, axis=-1)
    o = jnp.einsum('bhsm,bmhd->bshd', p.astype(mv.dtype), mv)
    return o.reshape(B, S, XQ_WIDTH)


def hierarchical_moe(h, w_group, b_group, w_router, b_router, w_gate, w_up, w_down):
    B, S, D = h.shape
    t = h.reshape(-1, D)
    T = t.shape[0]
    gl = (t @ w_group).astype(jnp.float32) + b_group.astype(jnp.float32)
    gp = jax.nn.softmax(gl, axis=-1)
    g_idx = jnp.argmax(gl, axis=-1)
    p_g = jnp.take_along_axis(gp, g_idx[:, None], axis=1)
    el = ((t @ w_router).astype(jnp.float32) + b_router.astype(jnp.float32)).reshape(
        T, N_GROUPS, EXPERTS_PER_GROUP)
    el = jnp.take_along_axis(el, g_idx[:, None, None], axis=1)[:, 0]
    top_v, top_i = lax.top_k(el, TOP_K_IN_GROUP)
    p_e = jax.nn.softmax(top_v, axis=-1)
    ids = g_idx[:, None] * EXPERTS_PER_GROUP + top_i
    combine = jnp.sum(jax.nn.one_hot(ids, N_EXPERTS, dtype=jnp.float32) * (p_g * p_e)[..., None],
                      axis=1).astype(h.dtype)
    y = jnp.zeros_like(t)
    for e in range(N_EXPERTS):
        hid = jax.nn.silu(t @ w_gate[e]) * (t @ w_up[e])
        y = y + combine[:, e:e + 1] * (hid @ w_down[e])
    return y.reshape(B, S, D)


def setup_inputs(seed: int = 0) -> dict:
    key = jax.random.key(seed)
    ks = iter(jax.random.split(key, 40))
    f32 = jnp.float32
    L = DEPTH

    def nrm(shape, scale):
        return jax.random.normal(next(ks), shape, f32) * scale

    def gain(shape):
        return 1.0 + 0.05 * jax.random.normal(next(ks), shape, f32)

    x = jax.random.normal(next(ks), (BATCH, SEQ, D_MODEL), f32)
    mem = jax.random.normal(next(ks), (BATCH, MEM_LEN, D_MODEL), f32)
    positions = jnp.broadcast_to(jnp.arange(SEQ, dtype=jnp.int32), (BATCH, SEQ))
    return {
        "x": x,
        "mem": mem,
        "positions": positions,
        "g_norm1": gain((L, D_MODEL)),
        "w_in": nrm((L, D_MODEL, IN_WIDTH), D_MODEL ** -0.5),
        "g_q": gain((L, HEAD_DIM)),
        "g_k": gain((L, HEAD_DIM)),
        "sinks": nrm((L, ATT_HEADS), 0.5),
        "w_o_attn": nrm((L, Q_WIDTH, D_MODEL), Q_WIDTH ** -0.5),
        "w_conv_dw": nrm((L, CONV_WIDTH, CONV_CH), CONV_WIDTH ** -0.5),
        "b_conv_dw": nrm((L, CONV_CH), 0.02),
        "g_conv_ln": gain((L, CONV_CH)),
        "b_conv_ln": nrm((L, CONV_CH), 0.02),
        "w_conv_out": nrm((L, CONV_CH, D_MODEL), CONV_CH ** -0.5),
        "g_mem": gain((L, D_MODEL)),
        "w_kv_mem": nrm((L, D_MODEL, 2 * XQ_WIDTH), D_MODEL ** -0.5),
        "g_xq": gain((L, MEM_HEAD_DIM)),
        "g_xk": gain((L, MEM_HEAD_DIM)),
        "w_o_mem": nrm((L, XQ_WIDTH, D_MODEL), XQ_WIDTH ** -0.5),
        "w_out": nrm((L, D_MODEL, D_MODEL), D_MODEL ** -0.5),
        "g_norm2": gain((L, D_MODEL)),
        "w_group": nrm((L, D_MODEL, N_GROUPS), D_MODEL ** -0.5),
        "b_group": nrm((L, N_GROUPS), 0.01),
        "w_router": nrm((L, D_MODEL, N_EXPERTS), D_MODEL ** -0.5),
        "b_router": nrm((L, N_EXPERTS), 0.01),
        "w_gate": nrm((L, N_EXPERTS, D_MODEL, EXPERT_FF), D_MODEL ** -0.5),
        "w_up": nrm((L, N_EXPERTS, D_MODEL, EXPERT_FF), D_MODEL ** -0.5),
        "w_down": nrm((L, N_EXPERTS, EXPERT_FF, D_MODEL), EXPERT_FF ** -0.5),
    }


def reference(x, mem, positions, g_norm1, w_in, g_q, g_k, sinks, w_o_attn, w_conv_dw, b_conv_dw,
              g_conv_ln, b_conv_ln, w_conv_out, g_mem, w_kv_mem, g_xq, g_xk, w_o_mem, w_out,
              g_norm2, w_group, b_group, w_router, b_router, w_gate, w_up, w_down):
    B, S, D = x.shape
    cos, sin = rope_tables(positions)
    for l in range(DEPTH):
        h = rms_norm(x, g_norm1[l])
        q, k, v, glu_in, xq, gate_logits = split_columns(
            h @ w_in[l], (Q_WIDTH, KV_WIDTH, KV_WIDTH, GLU_WIDTH, XQ_WIDTH, GATE_WIDTH))

        q = apply_rope(rms_norm(q.reshape(B, S, ATT_HEADS, HEAD_DIM), g_q[l]), cos, sin)
        k = apply_rope(rms_norm(k.reshape(B, S, ATT_KV_HEADS, HEAD_DIM), g_k[l]), cos, sin)
        v = v.reshape(B, S, ATT_KV_HEADS, HEAD_DIM)
        y_attn = sliding_window_attention(q, k, v, sinks[l]) @ w_o_attn[l]

        ga, gb = jnp.split(glu_in, 2, axis=-1)
        u = ga * jax.nn.sigmoid(gb)
        u = causal_depthwise_conv(u, w_conv_dw[l], b_conv_dw[l])
        u = jax.nn.silu(layer_norm(u, g_conv_ln[l], b_conv_ln[l]))
        y_conv = u @ w_conv_out[l]

        mem_n = rms_norm(mem, g_mem[l])
        y_mem = memory_cross_attention(xq, mem_n, w_kv_mem[l], g_xq[l], g_xk[l]) @ w_o_mem[l]

        gates = jax.nn.sigmoid(gate_logits.astype(jnp.float32)).astype(x.dtype).reshape(
            B, S, N_BRANCHES, D)
        merged = gates[:, :, 0] * y_attn + gates[:, :, 1] * y_conv + gates[:, :, 2] * y_mem
        x = x + merged @ w_out[l]

        h2 = rms_norm(x, g_norm2[l])
        x = x + hierarchical_moe(h2, w_group[l], b_group[l], w_router[l], b_router[l],
                                 w_gate[l], w_up[l], w_down[l])
    return x
```

```python
import math
import numpy as np
import concourse.bass as bass
import concourse.mybir as mybir
from concourse.bass_utils import run_bass_kernel_spmd

F32 = mybir.dt.float32
BF16 = mybir.dt.bfloat16
I32 = mybir.dt.int32
ACT = mybir.ActivationFunctionType
ALU = mybir.AluOpType
AX = mybir.AxisListType

D = 1024
NCORES = 8
MEM_LEN = 256
NE = 16
FF = 512
OFF_Q, OFF_K, OFF_V, OFF_GLU, OFF_XQ, OFF_GATE = 0, 512, 640, 768, 1792, 2304
IN_W = 5376

G1C, G2C, GMC, BDW, GLN, BLN, WDW = 0, 8, 16, 24, 28, 32, 36
GQK, GXQ, GXK, SINK, BRT, INVF, IDENT, MASKC, HFLAG = 160, 800, 1312, 1824, 1832, 1852, 1884, 2012, 2140
NCP = 2144

ENGS = ("sp", "act", "pool", "dve", "pe")
SAME_ENGINE_SYNC = True


class Buf:
    __slots__ = ("name", "last_write", "reads")

    def __init__(self, name=""):
        self.name = name
        self.last_write = None
        self.reads = {}


class Prog:
    def __init__(self, nc, same_engine_sync=True):
        self.nc = nc
        self.same_engine_sync = same_engine_sync
        self.ops = {e: [] for e in ENGS}
        self.sem = {}
        self.cnt = {}
        for e in ENGS:
            self.sem[e] = nc.alloc_semaphore("c_" + e)
            self.cnt[e] = 0
        self.waited = {e: {} for e in ENGS}
        self.n_dma_sems = 0
        self.disabled = False

    def dma_sem(self, name="d"):
        key = "dma_%s_%d" % (name, self.n_dma_sems)
        self.n_dma_sems += 1
        self.sem[key] = self.nc.alloc_semaphore(key)
        self.cnt[key] = 0
        return key

    def _collect(self, reads, writes):
        need = {}

        def add(k, v):
            if v > need.get(k, 0):
                need[k] = v
        for b in reads:
            if b.last_write is not None:
                add(*b.last_write)
        for b in writes:
            if b.last_write is not None:
                add(*b.last_write)
            for k, v in b.reads.items():
                add(k, v)
        return need

    def op(self, eng, fn, reads=(), writes=(), dma=None):
        if self.disabled:
            return 0
        need = self._collect(reads, writes)
        waits = []
        for k, v in need.items():
            if k == eng and dma is None:
                if eng == "pe" or not self.same_engine_sync:
                    continue
            if v > self.waited[eng].get(k, 0):
                waits.append((k, v))
                self.waited[eng][k] = v
        key = dma if dma is not None else eng
        inc = 16 if dma is not None else 1
        self.cnt[key] += inc
        val = self.cnt[key]
        self.ops[eng].append((waits, fn, key, inc))
        for b in writes:
            b.last_write = (key, val)
            b.reads = {}
        for b in reads:
            if b not in writes:
                if val > b.reads.get(key, 0):
                    b.reads[key] = val
        return val

    def barrier(self):
        if self.disabled:
            return
        for e in ENGS:
            waits = []
            for k, v in self.cnt.items():
                if k == e and v == 0:
                    continue
                if v > self.waited[e].get(k, 0):
                    waits.append((k, v))
                    self.waited[e][k] = v
            if waits:
                self.ops[e].append((waits, None, None, 0))

    def emit(self):
        nc = self.nc
        handles = {"sp": "sync", "act": "scalar", "pool": "gpsimd", "dve": "vector", "pe": "tensor"}
        with nc.Block() as block:
            for e in ENGS:
                ops = self.ops[e]

                def body(engh, ops=ops):
                    for waits, fn, key, inc in ops:
                        for k, v in waits:
                            engh.wait_ge(self.sem[k], v)
                        if fn is not None:
                            ins = fn(engh)
                            ins.then_inc(self.sem[key], inc)
                getattr(block, handles[e])(body)


def _dsize(dt):
    return {F32: 4, BF16: 2, I32: 4}[dt]


class Arena:
    def __init__(self, nc, nbytes):
        self.t = nc.alloc_sbuf_tensor("arena", [128, nbytes // 2], BF16)
        self.lo = 0
        self.hi = nbytes
        self.log = []

    def _view(self, off, shape, dt):
        n = 1
        for s in shape[1:]:
            n *= s
        size = n * _dsize(dt)
        ap = self.t[:, off // 2:(off + size) // 2]
        if dt != BF16:
            ap = ap.bitcast(dt)
        if len(shape) == 3:
            ap = ap.rearrange("p (a b) -> p a b", a=shape[1])
        elif len(shape) == 4:
            ap = ap.rearrange("p (a b c) -> p a b c", a=shape[1], b=shape[2])
        return ap

    def left(self, shape, dt):
        n = 1
        for s in shape[1:]:
            n *= s
        size = (n * _dsize(dt) + 63) // 64 * 64
        off = self.lo
        self.lo += size
        assert self.lo <= self.hi, "SBUF arena overflow (%d > %d)" % (self.lo, self.hi)
        self.log.append((off, size, tuple(shape)))
        return self._view(off, shape, dt)

    def right(self, shape, dt):
        n = 1
        for s in shape[1:]:
            n *= s
        size = (n * _dsize(dt) + 63) // 64 * 64
        self.hi -= size
        assert self.lo <= self.hi, "SBUF arena overflow"
        return self._view(self.hi, shape, dt)


def build_nc(NT=2048, debug=False, n_experts=NE, stop_after=None, NTP=1024, passes=None):
    NTP = min(NTP, NT)
    NPASS = NT // NTP
    NT_FULL = NT
    TT_FULL = NT // 128
    NT = NTP
    TT = NT // 128
    TT1 = TT + 1
    NS = NT // 512
    NTH = NT + 128
    nc = bass.Bass("TRN2", target_bir_lowering=False)

    def din(name, shape, dt=F32):
        return nc.dram_tensor(name, list(shape), dt, kind="ExternalInput").ap()

    x_d = din("x", [NT_FULL, D])
    xh_d = din("xh", [128, D])
    pos_d = din("pos", [128, TT_FULL + 1], I32)
    mem_d = din("mem", [MEM_LEN, D])
    cp_d = din("cp", [128, NCP])
    w_in_d = din("w_in", [D, IN_W])
    woa_d = din("w_o_attn", [512, D])
    wco_d = din("w_conv_out", [512, D])
    wkv_d = din("w_kv_mem", [D, 1024])
    wom_d = din("w_o_mem", [512, D])
    wout_d = din("w_out", [D, D])
    wgr_d = din("w_gr", [D, 20])
    wg_d = din("w_gate", [NE, D, FF])
    wu_d = din("w_up", [NE, D, FF])
    wd_d = din("w_down", [NE, FF, D])
    out_d = nc.dram_tensor("out", [NT_FULL, D], F32, kind="ExternalOutput").ap()
    dbg = {}
    if debug:
        dbg["x1"] = nc.dram_tensor("d_x1", [NT, D], F32, kind="ExternalOutput").ap()
        dbg["call"] = nc.dram_tensor("d_call", [128, TT * 16], F32, kind="ExternalOutput").ap()
        dbg["merged"] = nc.dram_tensor("d_merged", [128, 8 * NT], BF16, kind="ExternalOutput").ap()
        dbg["hT"] = nc.dram_tensor("d_hT", [128, 8 * NTH], BF16, kind="ExternalOutput").ap()

    P = Prog(nc, same_engine_sync=SAME_ENGINE_SYNC)
    posi_t = nc.alloc_sbuf_tensor("posi", [128, TT1], I32)
    ki_t = nc.alloc_sbuf_tensor("ki", [128, TT1, 32], I32)
    AR = Arena(nc, 229344 - 16512 - 64 - 2048)
    arena_hi0 = AR.hi
    PS = [nc.alloc_psum_tensor("ps%d" % i, [128, 512], F32) for i in range(8)]
    PB = [Buf("ps%d" % i) for i in range(8)]

    def maybe_stop(tag):
        if stop_after == tag and not P.disabled:
            P.barrier()
            if debug:
                sd = P.dma_sem("dbgstop")
                bd = Buf("dbgstop")
                P.op("sp", lambda e: e.dma_start(out=dbg["hT"], in_=hT.rearrange("p a b -> p (a b)")), writes=[bd], dma=sd)
                P.op("sp", lambda e: e.dma_start(out=dbg["merged"], in_=merged.rearrange("p a b -> p (a b)")), writes=[bd], dma=sd)
            P.barrier()
            P.disabled = True

    def psf(i):
        return PS[i][:, :]

    def psb(i):
        return PS[i][:, :].bitcast(BF16)

    cp = AR.left([128, NCP], F32)
    b_cp = Buf("cp")
    identb = AR.left([128, 128], BF16)
    maskc4 = AR.left([128, 4, 128], BF16)
    maskp4 = AR.left([128, 4, 128], BF16)
    ones_f = AR.left([128, 128], F32)
    ones_b = AR.left([128, 128], BF16)
    esink = AR.left([128, 8], F32)
    eps6 = AR.left([128, 1], F32)
    eps5 = AR.left([128, 1], F32)
    cos_t = AR.left([128, TT1, 32], F32)
    sin_t = AR.left([128, TT1, 32], F32)
    c_all = AR.left([128, TT, 16], F32)
    b_const = Buf("const")
    b_call = Buf("call")
    identf = cp[:, IDENT:IDENT + 128]

    s_cp = P.dma_sem("cp")
    P.op("sp", lambda e: e.dma_start(out=cp, in_=cp_d), writes=[b_cp], dma=s_cp)
    P.op("dve", lambda e: e.tensor_copy(out=identb, in_=identf), reads=[b_cp], writes=[b_const])
    mk_b = cp[:, MASKC:MASKC + 128].unsqueeze(1).to_broadcast([128, 4, 128])
    P.op("dve", lambda e: e.tensor_scalar(out=maskc4, in0=mk_b, scalar1=30000.0, scalar2=-30000.0, op0=ALU.mult, op1=ALU.add),
         reads=[b_cp], writes=[b_const])
    P.op("dve", lambda e: e.tensor_scalar(out=maskp4, in0=mk_b, scalar1=-30000.0, scalar2=None, op0=ALU.mult),
         reads=[b_cp], writes=[b_const])
    P.op("pool", lambda e: e.memset(ones_f, 1.0), writes=[b_const])
    P.op("pool", lambda e: e.memset(ones_b, 1.0), writes=[b_const])
    P.op("pool", lambda e: e.memset(eps6, 1e-6), writes=[b_const])
    P.op("pool", lambda e: e.memset(eps5, 1e-5), writes=[b_const])
    P.op("act", lambda e: e.activation(out=esink, in_=cp[:, SINK:SINK + 8], func=ACT.Exp), reads=[b_cp], writes=[b_const])

    WSIZE = 53248 + 128
    wstate = {"base": AR.lo, "ptr": AR.lo}
    AR.lo += WSIZE

    def W(shape, dt):
        save = AR.lo
        AR.lo = wstate["ptr"]
        ap = AR.left(shape, dt)
        wstate["ptr"] = AR.lo
        assert wstate["ptr"] <= wstate["base"] + WSIZE, "W region overflow"
        AR.lo = save
        return ap

    def Wreset():
        wstate["ptr"] = wstate["base"]

    mark0 = AR.lo

    def run_pass(pi):
        Wreset()
        wkv = W([128, 8, 1024], BF16)
        b_wkv = Buf("wkv")
        s_wkv = P.dma_sem("wkv")
        wA = W([128, 8, 768], BF16)
        b_wA = Buf("wA")
        s_wA = P.dma_sem("wA")
        P.op("pool", lambda e: e.dma_start(out=wkv, in_=wkv_d.rearrange("(kc p) n -> p kc n", p=128)), writes=[b_wkv], dma=s_wkv)
        P.op("pool", lambda e: e.dma_start(out=wA, in_=w_in_d[:, 0:768].rearrange("(kc p) n -> p kc n", p=128)), writes=[b_wA], dma=s_wA)
        posi = posi_t[:, :]
        posf = W([128, TT1], F32)
        ang = W([128, TT1, 32], F32)
        rr = [W([128, TT1, 32], F32) for _ in range(4)]
        ki = ki_t[:, :, :]
        b_r = Buf("rope")
        s_pos = P.dma_sem("pos")
        P.op("sp", lambda e: e.dma_start(out=posi, in_=pos_d[:, pi * TT:pi * TT + TT1]), writes=[b_r], dma=s_pos)
        P.op("dve", lambda e: e.tensor_copy(out=posf, in_=posi), reads=[b_r], writes=[b_r])
        P.op("dve", lambda e: e.tensor_tensor(out=ang, in0=posf.unsqueeze(2).to_broadcast([128, TT1, 32]),
                                              in1=cp[:, INVF:INVF + 32].unsqueeze(1).to_broadcast([128, TT1, 32]), op=ALU.mult),
             reads=[b_r, b_cp], writes=[b_r])
        TWO_PI = 2.0 * math.pi
        C1 = 6.28125
        C2 = TWO_PI - C1
        P.op("dve", lambda e: e.tensor_scalar(out=rr[0], in0=ang, scalar1=1.0 / TWO_PI, scalar2=None, op0=ALU.mult), reads=[b_r], writes=[b_r])
        P.op("dve", lambda e: e.tensor_copy(out=ki, in_=rr[0]), reads=[b_r], writes=[b_r])
        P.op("dve", lambda e: e.tensor_copy(out=rr[0], in_=ki), reads=[b_r], writes=[b_r])
        P.op("dve", lambda e: e.scalar_tensor_tensor(out=rr[1], in0=rr[0], scalar=-C1, in1=ang, op0=ALU.mult, op1=ALU.add), reads=[b_r], writes=[b_r])
        P.op("dve", lambda e: e.scalar_tensor_tensor(out=rr[2], in0=rr[0], scalar=-C2, in1=rr[1], op0=ALU.mult, op1=ALU.add), reads=[b_r], writes=[b_r])
        PI_T = 3.1415925

        def wrap_sin(dst, shift):
            P.op("dve", lambda e: e.tensor_scalar(out=rr[1], in0=rr[2], scalar1=shift, scalar2=None, op0=ALU.add), reads=[b_r], writes=[b_r])
            P.op("dve", lambda e: e.tensor_scalar(out=rr[0], in0=rr[1], scalar1=PI_T, scalar2=None, op0=ALU.is_gt), reads=[b_r], writes=[b_r])
            P.op("dve", lambda e: e.scalar_tensor_tensor(out=rr[3], in0=rr[0], scalar=-TWO_PI, in1=rr[1], op0=ALU.mult, op1=ALU.add), reads=[b_r], writes=[b_r])
            P.op("dve", lambda e: e.tensor_scalar(out=rr[0], in0=rr[3], scalar1=-PI_T, scalar2=None, op0=ALU.is_lt), reads=[b_r], writes=[b_r])
            P.op("dve", lambda e: e.scalar_tensor_tensor(out=rr[1], in0=rr[0], scalar=TWO_PI, in1=rr[3], op0=ALU.mult, op1=ALU.add), reads=[b_r], writes=[b_r])
            P.op("dve", lambda e: e.tensor_scalar(out=rr[3], in0=rr[1], scalar1=-3.141592, scalar2=3.141592, op0=ALU.max, op1=ALU.min), reads=[b_r], writes=[b_r])
            P.op("act", lambda e: e.activation(out=dst, in_=rr[3], func=ACT.Sin), reads=[b_r], writes=[b_const])

        wrap_sin(sin_t, 0.0)
        wrap_sin(cos_t, math.pi / 2)
        AR.lo = mark0

        merged = AR.left([128, 8, NT], BF16)
        hT = AR.left([128, 8, NTH], BF16)
        bT = AR.left([128, 4, NT], BF16)
        mkT = AR.left([128, 4, MEM_LEN], BF16)
        mv_sb = AR.left([128, 2, 512], BF16)
        rstd_all = AR.left([128, TT1 + 2], F32)
        ss_all = AR.left([128, TT1 + 2], F32)
        b_hT = [Buf("hT%d" % t) for t in range(TT1)]
        b_merged = [[Buf() for _ in range(NS)] for _ in range(8)]
        b_bT = [Buf("bT%d" % t) for t in range(TT)]
        b_wo = Buf("wo")
        s_wo = P.dma_sem("wo")
        b_gw = [Buf("gw0"), Buf("gw1")]
        s_gw = [P.dma_sem("gw0"), P.dma_sem("gw1")]
        b_memT = Buf("memT")
        b_mkT = Buf("mkT")
        b_mv = Buf("mv")
        b_rstd = Buf("rstd")
        b_ss = Buf("ss")
        mark1 = AR.lo

        memT = AR.left([128, 8, MEM_LEN], BF16)
        mark1b = AR.lo
        xt = [AR.left([128, D], F32) for _ in range(3)]
        b_xt = [Buf("xt%d" % i) for i in range(3)]
        s_xt = [P.dma_sem("xt%d" % i) for i in range(3)]
        junk = AR.left([128, D], BF16)
        b_junk = Buf("junk")
        xn = [AR.left([128, D], BF16) for _ in range(2)]
        b_xn = [Buf("xn0"), Buf("xn1")]
        lnt = AR.left([128, TT1 + 2], F32)
        NTILES = TT1 + 2

        def src_tile(i):
            if i == 0:
                return xh_d if pi == 0 else x_d[pi * NT - 128:pi * NT, :]
            if i <= TT:
                return x_d[pi * NT + (i - 1) * 128:pi * NT + i * 128, :]
            j = i - TT1
            return mem_d[j * 128:(j + 1) * 128, :]

        g1b = cp[:, G1C:G1C + 8].unsqueeze(2).to_broadcast([128, 8, 128])
        gmb = cp[:, GMC:GMC + 8].unsqueeze(2).to_broadcast([128, 8, 128])
        b_rs = [Buf() for _ in range(NTILES)]
        for i in range(NTILES):
            k = i % 3
            q2 = i % 2
            P.op("sp", lambda e, k=k, i=i: e.dma_start(out=xt[k], in_=src_tile(i)), writes=[b_xt[k]], dma=s_xt[k])
            P.op("act", lambda e, k=k, i=i: e.activation(out=junk, in_=xt[k], func=ACT.Square, accum_out=ss_all[:, i:i + 1]),
                 reads=[b_xt[k]], writes=[b_junk, b_rs[i]])
            P.op("act", lambda e, i=i: e.activation(out=lnt[:, i:i + 1], in_=ss_all[:, i:i + 1], func=ACT.Ln, scale=1.0 / D, bias=eps6[:, 0:1]),
                 reads=[b_const], writes=[b_rs[i]])
            P.op("act", lambda e, i=i: e.activation(out=rstd_all[:, i:i + 1], in_=lnt[:, i:i + 1], func=ACT.Exp, scale=-0.5), writes=[b_rs[i]])
            P.op("dve", lambda e, k=k, i=i, q2=q2: e.tensor_scalar(out=xn[q2], in0=xt[k], scalar1=rstd_all[:, i:i + 1], scalar2=None, op0=ALU.mult),
                 reads=[b_xt[k], b_rs[i]], writes=[b_xn[q2]])

            def tr(e, q2=q2):
                ins = None
                for c in range(8):
                    ins = e.transpose(out=psb(q2)[:, c * 128:(c + 1) * 128], in_=xn[q2][:, c * 128:(c + 1) * 128], identity=identb)
                return ins
            P.op("pe", tr, reads=[b_xn[q2], b_const], writes=[PB[q2]])
            src = psb(q2).rearrange("p (c t) -> p c t", c=8)
            if i <= TT:
                P.op("dve", lambda e, i=i, src=src: e.tensor_tensor(out=hT[:, :, i * 128:(i + 1) * 128], in0=src, in1=g1b, op=ALU.mult),
                     reads=[PB[q2], b_cp], writes=[b_hT[i]])
            else:
                j = i - TT1
                P.op("dve", lambda e, j=j, src=src: e.tensor_tensor(out=memT[:, :, j * 128:(j + 1) * 128], in0=src, in1=gmb, op=ALU.mult),
                     reads=[PB[q2], b_cp], writes=[b_memT])
        P.barrier()
        maybe_stop("p1a")
        AR.lo = mark1b

        sqm = AR.left([128, 512], F32)
        t1m = AR.left([128, 512], F32)
        mkn = AR.left([128, 512], BF16)
        ssk = AR.left([128, 4], F32)
        lk = AR.left([128, 4], F32)
        rk = AR.left([128, 4], F32)
        b_m = Buf("memscratch")
        for mt in range(2):
            def mm(e, mt=mt):
                ins = None
                for half in range(2):
                    for kc in range(8):
                        ins = e.matmul(out=psf(half), lhsT=memT[:, kc, mt * 128:(mt + 1) * 128], rhs=wkv[:, kc, half * 512:(half + 1) * 512],
                                       start=(kc == 0), stop=(kc == 7))
                return ins
            P.op("pe", mm, reads=[b_memT, b_wkv], writes=[PB[0], PB[1]])
            P.op("act", lambda e: e.activation(out=sqm, in_=psf(0), func=ACT.Square), reads=[PB[0]], writes=[b_m])
            P.op("dve", lambda e: e.tensor_reduce(out=ssk, in_=sqm.rearrange("p (h d) -> p h d", h=4), axis=AX.X, op=ALU.add), reads=[b_m], writes=[b_m])
            P.op("act", lambda e: e.activation(out=lk, in_=ssk, func=ACT.Ln, scale=1.0 / 128, bias=eps6[:, 0:1]), reads=[b_m, b_const], writes=[b_m])
            P.op("act", lambda e: e.activation(out=rk, in_=lk, func=ACT.Exp, scale=-0.5), reads=[b_m], writes=[b_m])
            P.op("dve", lambda e: e.tensor_tensor(out=t1m.rearrange("p (h d) -> p h d", h=4), in0=psf(0).rearrange("p (h d) -> p h d", h=4),
                                                  in1=rk.unsqueeze(2).to_broadcast([128, 4, 128]), op=ALU.mult), reads=[PB[0], b_m], writes=[b_m])
            P.op("dve", lambda e: e.tensor_tensor(out=mkn, in0=t1m, in1=cp[:, GXK:GXK + 512], op=ALU.mult), reads=[b_m, b_cp], writes=[b_m])

            def trk(e):
                ins = None
                for h in range(4):
                    ins = e.transpose(out=psb(2)[:, h * 128:(h + 1) * 128], in_=mkn[:, h * 128:(h + 1) * 128], identity=identb)
                return ins
            P.op("pe", trk, reads=[b_m, b_const], writes=[PB[2]])
            P.op("act", lambda e, mt=mt: e.activation(out=mkT[:, :, mt * 128:(mt + 1) * 128], in_=psb(2)[:, 0:512].rearrange("p (h t) -> p h t", h=4), func=ACT.Copy),
                 reads=[PB[2]], writes=[b_mkT])
            P.op("act", lambda e, mt=mt: e.activation(out=mv_sb[:, mt, :], in_=psf(1), func=ACT.Copy), reads=[PB[1]], writes=[b_mv])
        P.barrier()
        maybe_stop("p1b")
        AR.lo = mark1

        def cast_dma(dst, src, b, s):
            P.op("pool", lambda e: e.dma_start(out=dst, in_=src), writes=[b], dma=s)

        def branch_merge(w_o_d, goff, first):
            Wreset()
            wo = W([128, 4, 1024], BF16)
            gw = [W([128, 8, 256], BF16) for _ in range(2)]
            cast_dma(wo, w_o_d.rearrange("(kc p) n -> p kc n", p=128), b_wo, s_wo)
            tmp = [AR.left([128, 512], F32) for _ in range(2)]
            sg = [AR.left([128, 512], F32) for _ in range(2)]
            b_tmp = [Buf(), Buf()]
            b_sg = [Buf(), Buf()]
            it = 0
            for s in range(4):
                gb_ = s % 2
                cast_dma(gw[gb_], w_in_d[:, goff + s * 256:goff + (s + 1) * 256].rearrange("(kc p) n -> p kc n", p=128), b_gw[gb_], s_gw[gb_])
                for mi in range(2):
                    m = 2 * s + mi
                    for n in range(NS):
                        pp = it % 2
                        it += 1
                        yb, gbk = 4 + pp, 6 + pp

                        def mmy(e, m=m, n=n, yb=yb):
                            ins = None
                            for kc in range(4):
                                ins = e.matmul(out=psf(yb), lhsT=wo[:, kc, m * 128:(m + 1) * 128], rhs=bT[:, kc, n * 512:(n + 1) * 512],
                                               start=(kc == 0), stop=(kc == 3))
                            return ins
                        P.op("pe", mmy, reads=[b_wo] + b_bT[n * 4:(n + 1) * 4], writes=[PB[yb]])

                        def mmg(e, mi=mi, n=n, gbk=gbk, gb_=gb_):
                            ins = None
                            for kc in range(8):
                                ins = e.matmul(out=psf(gbk), lhsT=gw[gb_][:, kc, mi * 128:(mi + 1) * 128],
                                               rhs=hT[:, kc, 128 + n * 512:128 + (n + 1) * 512], start=(kc == 0), stop=(kc == 7))
                            return ins
                        P.op("pe", mmg, reads=[b_gw[gb_]] + b_hT[1 + n * 4:1 + (n + 1) * 4], writes=[PB[gbk]])
                        P.op("act", lambda e, gbk=gbk, pp=pp: e.activation(out=sg[pp], in_=psf(gbk), func=ACT.Sigmoid), reads=[PB[gbk]], writes=[b_sg[pp]])
                        dst = merged[:, m, n * 512:(n + 1) * 512]
                        if first:
                            P.op("dve", lambda e, yb=yb, pp=pp, dst=dst: e.tensor_tensor(out=dst, in0=psf(yb), in1=sg[pp], op=ALU.mult),
                                 reads=[PB[yb], b_sg[pp]], writes=[b_merged[m][n]])
                        else:
                            P.op("dve", lambda e, yb=yb, pp=pp: e.tensor_tensor(out=tmp[pp], in0=psf(yb), in1=sg[pp], op=ALU.mult),
                                 reads=[PB[yb], b_sg[pp]], writes=[b_tmp[pp]])
                            P.op("dve", lambda e, pp=pp, dst=dst: e.tensor_tensor(out=dst, in0=dst, in1=tmp[pp], op=ALU.add),
                                 reads=[b_tmp[pp]], writes=[b_merged[m][n]])

        kT_all = AR.left([128, TT1 * 128], BF16)
        vaug = AR.left([128, TT1, 2, 65], BF16)
        b_kT = [Buf() for _ in range(TT1)]
        b_v = [Buf() for _ in range(TT1)]
        NB = 3
        sqs = [AR.left([128, 640], F32) for _ in range(NB)]
        ss10 = [AR.left([128, 10], F32) for _ in range(NB)]
        l10 = [AR.left([128, 10], F32) for _ in range(NB)]
        r10 = [AR.left([128, 10], F32) for _ in range(NB)]
        t1 = [AR.left([128, 10, 64], F32) for _ in range(NB)]
        t2 = [AR.left([128, 10, 64], F32) for _ in range(NB)]
        ra = [AR.left([128, 10, 32], F32) for _ in range(NB)]
        rb = [AR.left([128, 10, 32], F32) for _ in range(NB)]
        rc = [AR.left([128, 10, 32], F32) for _ in range(NB)]
        rd = [AR.left([128, 10, 32], F32) for _ in range(NB)]
        qkb = [AR.left([128, 640], BF16) for _ in range(NB)]
        qT = [AR.left([128, 512], BF16) for _ in range(2)]
        b_qT = [Buf(), Buf()]
        Pt = [AR.left([128, 512], BF16) for _ in range(2)]
        Pm = [AR.left([128, 512], BF16) for _ in range(4)]
        b_Pt = [Buf(), Buf()]
        b_Pm = [Buf() for _ in range(4)]
        den = [AR.left([128, 8], F32) for _ in range(NB)]
        rden = [AR.left([128, 8], F32) for _ in range(NB)]
        On = [AR.left([128, 8, 64], BF16) for _ in range(NB)]
        b_sq = [Buf() for _ in range(NB)]
        b_r10 = [Buf() for _ in range(NB)]
        b_t1 = [Buf() for _ in range(NB)]
        b_t2 = [Buf() for _ in range(NB)]
        b_ra = [Buf() for _ in range(NB)]
        b_rb = [Buf() for _ in range(NB)]
        b_rc = [Buf() for _ in range(NB)]
        b_rd = [Buf() for _ in range(NB)]
        b_qkb = [Buf() for _ in range(NB)]
        b_On = [Buf() for _ in range(NB)]
        b_den = [Buf() for _ in range(NB)]

        P.op("pool", lambda e: e.memset(vaug[:, :, :, 64:65], 1.0), writes=b_v)
        P.op("dve", lambda e: e.tensor_copy(out=vaug[:, 0, :, 64:65], in_=(cp[:, HFLAG:HFLAG + 1] if pi == 0 else ones_f[:, 0:1]).unsqueeze(1).to_broadcast([128, 2, 1])),
             reads=[b_cp], writes=[b_v[0]])
        QB, KVB, TB6, TB7 = 0, 1, 6, 7

        def stageA1(t):
            z = t % NB

            def mmq(e):
                ins = None
                for kc in range(8):
                    ins = e.matmul(out=psf(QB), lhsT=hT[:, kc, t * 128:(t + 1) * 128], rhs=wA[:, kc, 0:512], start=(kc == 0), stop=(kc == 7))
                for kc in range(8):
                    ins = e.matmul(out=psf(KVB)[:, 0:256], lhsT=hT[:, kc, t * 128:(t + 1) * 128], rhs=wA[:, kc, 512:768], start=(kc == 0), stop=(kc == 7))
                return ins
            P.op("pe", mmq, reads=[b_hT[t], b_wA], writes=[PB[QB], PB[KVB]])
            P.op("act", lambda e: e.activation(out=sqs[z][:, 0:512], in_=psf(QB), func=ACT.Square), reads=[PB[QB]], writes=[b_sq[z]])
            P.op("act", lambda e: e.activation(out=sqs[z][:, 512:640], in_=psf(KVB)[:, 0:128], func=ACT.Square), reads=[PB[KVB]], writes=[b_sq[z]])
            P.op("act", lambda e: e.activation(out=vaug[:, t, :, 0:64], in_=psf(KVB)[:, 128:256].rearrange("p (g d) -> p g d", g=2), func=ACT.Copy),
                 reads=[PB[KVB]], writes=[b_v[t]])
            P.op("dve", lambda e: e.tensor_reduce(out=ss10[z], in_=sqs[z].rearrange("p (h d) -> p h d", h=10), axis=AX.X, op=ALU.add), reads=[b_sq[z]], writes=[b_r10[z]])
            P.op("act", lambda e: e.activation(out=l10[z], in_=ss10[z], func=ACT.Ln, scale=1.0 / 64, bias=eps6[:, 0:1]), reads=[b_r10[z], b_const], writes=[b_r10[z]])
            P.op("act", lambda e: e.activation(out=r10[z], in_=l10[z], func=ACT.Exp, scale=-0.5), reads=[b_r10[z]], writes=[b_r10[z]])
            P.op("dve", lambda e: e.tensor_tensor(out=t1[z][:, 0:8, :], in0=psf(QB).rearrange("p (h d) -> p h d", h=8),
                                                  in1=r10[z][:, 0:8].unsqueeze(2).to_broadcast([128, 8, 64]), op=ALU.mult), reads=[PB[QB], b_r10[z]], writes=[b_t1[z]])
            P.op("dve", lambda e: e.tensor_tensor(out=t1[z][:, 8:10, :], in0=psf(KVB)[:, 0:128].rearrange("p (h d) -> p h d", h=2),
                                                  in1=r10[z][:, 8:10].unsqueeze(2).to_broadcast([128, 2, 64]), op=ALU.mult), reads=[PB[KVB], b_r10[z]], writes=[b_t1[z]])
            P.op("pool", lambda e: e.tensor_tensor(out=t2[z], in0=t1[z], in1=cp[:, GQK:GQK + 640].rearrange("p (h d) -> p h d", h=10), op=ALU.mult),
                 reads=[b_t1[z], b_cp], writes=[b_t2[z]])
            cb = cos_t[:, t, :].unsqueeze(1).to_broadcast([128, 10, 32])
            sb = sin_t[:, t, :].unsqueeze(1).to_broadcast([128, 10, 32])
            P.op("dve", lambda e: e.tensor_tensor(out=ra[z], in0=t2[z][:, :, 0:32], in1=cb, op=ALU.mult), reads=[b_t2[z], b_const], writes=[b_ra[z]])
            P.op("pool", lambda e: e.tensor_tensor(out=rb[z], in0=t2[z][:, :, 32:64], in1=sb, op=ALU.mult), reads=[b_t2[z], b_const], writes=[b_rb[z]])
            P.op("dve", lambda e: e.tensor_tensor(out=rc[z], in0=t2[z][:, :, 32:64], in1=cb, op=ALU.mult), reads=[b_t2[z], b_const], writes=[b_rc[z]])
            P.op("pool", lambda e: e.tensor_tensor(out=rd[z], in0=t2[z][:, :, 0:32], in1=sb, op=ALU.mult), reads=[b_t2[z], b_const], writes=[b_rd[z]])
            qdst = qkb[z][:, 0:512].rearrange("p (c g d) -> p g c d", c=4, g=2)
            kdst = qkb[z][:, 512:640].rearrange("p (h d) -> p h d", h=2)

            def v4(a):
                return a[:, 0:8, :].rearrange("p (g c) d -> p g c d", g=2)
            P.op("dve", lambda e: e.tensor_tensor(out=qdst[:, :, :, 0:32], in0=v4(ra[z]), in1=v4(rb[z]), op=ALU.subtract), reads=[b_ra[z], b_rb[z]], writes=[b_qkb[z]])
            P.op("dve", lambda e: e.tensor_tensor(out=qdst[:, :, :, 32:64], in0=v4(rc[z]), in1=v4(rd[z]), op=ALU.add), reads=[b_rc[z], b_rd[z]], writes=[b_qkb[z]])
            P.op("dve", lambda e: e.tensor_tensor(out=kdst[:, :, 0:32], in0=ra[z][:, 8:10, :], in1=rb[z][:, 8:10, :], op=ALU.subtract), reads=[b_ra[z], b_rb[z]], writes=[b_qkb[z]])
            P.op("dve", lambda e: e.tensor_tensor(out=kdst[:, :, 32:64], in0=rc[z][:, 8:10, :], in1=rd[z][:, 8:10, :], op=ALU.add), reads=[b_rc[z], b_rd[z]], writes=[b_qkb[z]])

        def stageA2(t):
            z = t % NB
            qq = t % 2

            def trq(e):
                ins = None
                for c in range(5):
                    ins = e.transpose(out=psb(TB6)[:, c * 128:(c + 1) * 128], in_=qkb[z][:, c * 128:(c + 1) * 128], identity=identb)
                return ins
            P.op("pe", trq, reads=[b_qkb[z], b_const], writes=[PB[TB6]])
            P.op("act", lambda e: e.activation(out=qT[qq], in_=psb(TB6)[:, 0:512], func=ACT.Copy), reads=[PB[TB6]], writes=[b_qT[qq]])
            P.op("dve", lambda e: e.tensor_copy(out=kT_all[:, t * 128:(t + 1) * 128], in_=psb(TB6)[:, 512:640]), reads=[PB[TB6]], writes=[b_kT[t]])

        def stageB(t):
            z = t % NB
            qq = t % 2

            def rec_S(g, jj):
                j = (t - 1, t)[jj]
                sbk = 2 + jj
                pt = jj
                pm = 2 * g + jj
                msk = (maskp4 if jj == 0 else maskc4)

                def mms(e):
                    e.matmul(out=psf(sbk), lhsT=kT_all[g * 64:(g + 1) * 64, j * 128:(j + 1) * 128],
                             rhs=qT[qq][g * 64:(g + 1) * 64, :], start=True, stop=False)
                    return e.matmul(out=psf(sbk), lhsT=identb, rhs=msk.rearrange("p a b -> p (a b)"), start=False, stop=True)
                P.op("pe", mms, reads=[b_kT[j], b_qT[qq], b_const], writes=[PB[sbk]])
                P.op("act", lambda e: e.activation(out=Pm[pm], in_=psf(sbk), func=ACT.Exp, scale=0.125), reads=[PB[sbk]], writes=[b_Pm[pm]])

            def rec_PV(g, jj):
                j = (t - 1, t)[jj]
                pm = 2 * g + jj
                ob = 4 + g

                def pv(e):
                    ins = None
                    for c in range(4):
                        ins = e.matmul(out=psf(ob)[:, c * 65:(c + 1) * 65], lhsT=Pm[pm][:, c * 128:(c + 1) * 128], rhs=vaug[:, j, g, :],
                                       start=(jj == 0 and c == 0), stop=(jj == 1 and c == 3), skip_group_check=True)
                    return ins
                P.op("pe", pv, reads=[b_Pm[pm], b_v[j]], writes=[PB[ob]])
            rec_S(0, 0)
            rec_S(0, 1)
            rec_PV(0, 0)
            rec_S(1, 0)
            rec_PV(0, 1)
            rec_S(1, 1)
            rec_PV(1, 0)
            rec_PV(1, 1)
            for g in range(2):
                ob = 4 + g
                ov = psf(ob)[:, 0:260].rearrange("p (c d) -> p c d", c=4)
                P.op("dve", lambda e, g=g, ov=ov: e.tensor_tensor(out=den[z][:, g * 4:(g + 1) * 4], in0=ov[:, :, 64], in1=esink[:, g * 4:(g + 1) * 4], op=ALU.add),
                     reads=[PB[ob], b_const], writes=[b_den[z]])
            P.op("dve", lambda e: e.reciprocal(out=rden[z], in_=den[z]), reads=[b_den[z]], writes=[b_den[z]])
            for g in range(2):
                ob = 4 + g
                ov = psf(ob)[:, 0:260].rearrange("p (c d) -> p c d", c=4)
                P.op("dve", lambda e, g=g, ov=ov: e.tensor_tensor(out=On[z][:, g * 4:(g + 1) * 4, :], in0=ov[:, :, 0:64],
                                                                  in1=rden[z][:, g * 4:(g + 1) * 4].unsqueeze(2).to_broadcast([128, 4, 64]), op=ALU.mult),
                     reads=[PB[ob], b_den[z]], writes=[b_On[z]])

            def tro(e):
                ins = None
                of = On[z].rearrange("p h d -> p (h d)")
                for c in range(4):
                    ins = e.transpose(out=psb(TB7)[:, c * 128:(c + 1) * 128], in_=of[:, c * 128:(c + 1) * 128], identity=identb)
                return ins
            P.op("pe", tro, reads=[b_On[z], b_const], writes=[PB[TB7]])
            P.op("act", lambda e: e.activation(out=bT[:, :, (t - 1) * 128:t * 128], in_=psb(TB7)[:, 0:512].rearrange("p (c t) -> p c t", c=4), func=ACT.Copy),
                 reads=[PB[TB7]], writes=[b_bT[t - 1]])

        stageA1(0)
        stageA1(1)
        stageA2(0)
        if TT1 > 2:
            stageA1(2)
        stageA2(1)
        for t in range(1, TT1):
            if t + 2 < TT1:
                stageA1(t + 2)
            stageB(t)
            if t + 1 < TT1:
                stageA2(t + 1)
        P.barrier()
        maybe_stop("attn")
        mark_att = AR.lo
        AR.lo = mark1
        branch_merge(woa_d, OFF_GATE, True)
        P.barrier()
        maybe_stop("attn_merge")
        AR.lo = mark1

        Wreset()
        diag = W([128, 4, 31, 128], BF16)
        b_diag = [Buf() for _ in range(4)]
        for c in range(4):
            eng = "dve" if c % 2 == 0 else "pool"
            P.op(eng, lambda e, c=c: e.tensor_tensor(out=diag[:, c], in0=identb.unsqueeze(1).to_broadcast([128, 31, 128]),
                                                     in1=cp[:, WDW + c * 31:WDW + (c + 1) * 31].unsqueeze(2).to_broadcast([128, 31, 128]), op=ALU.mult),
                 reads=[b_const, b_cp], writes=[b_diag[c]])
        w_after_diag = wstate["ptr"]
        uT = AR.left([128, 4, NTH], BF16)
        b_uT = [[Buf() for _ in range(NS + 1)] for _ in range(4)]
        mark_cv = AR.lo
        wB = [W([128, 8, 256], BF16) for _ in range(2)]
        b_wB = [Buf(), Buf()]
        s_wB = [P.dma_sem("wB0"), P.dma_sem("wB1")]
        sgb = [AR.left([128, 512], F32) for _ in range(2)]
        b_sgb = [Buf(), Buf()]
        segs = [(0, 128)] + [(128 + n * 512, 512) for n in range(NS)]
        it = 0
        for c in range(4):
            wb = c % 2
            P.op("pool", lambda e, c=c, wb=wb: e.dma_start(out=wB[wb][:, :, 0:128],
                                                           in_=w_in_d[:, OFF_GLU + c * 128:OFF_GLU + (c + 1) * 128].rearrange("(kc p) n -> p kc n", p=128)),
                 writes=[b_wB[wb]], dma=s_wB[wb])
            P.op("pool", lambda e, c=c, wb=wb: e.dma_start(out=wB[wb][:, :, 128:256],
                                                           in_=w_in_d[:, OFF_GLU + 512 + c * 128:OFF_GLU + 512 + (c + 1) * 128].rearrange("(kc p) n -> p kc n", p=128)),
                 writes=[b_wB[wb]], dma=s_wB[wb])
            for si, (st, ln) in enumerate(segs):
                pp = it % 2
                it += 1
                ga, gbk = 0 + pp, 2 + pp
                hb = [b_hT[0]] if si == 0 else b_hT[1 + (si - 1) * 4:1 + si * 4]

                def mmglu(e, wb=wb, st=st, ln=ln, ga=ga, gbk=gbk):
                    ins = None
                    for kc in range(8):
                        ins = e.matmul(out=psf(ga)[:, 0:ln], lhsT=wB[wb][:, kc, 0:128], rhs=hT[:, kc, st:st + ln], start=(kc == 0), stop=(kc == 7))
                    for kc in range(8):
                        ins = e.matmul(out=psf(gbk)[:, 0:ln], lhsT=wB[wb][:, kc, 128:256], rhs=hT[:, kc, st:st + ln], start=(kc == 0), stop=(kc == 7))
                    return ins
                P.op("pe", mmglu, reads=[b_wB[wb]] + hb, writes=[PB[ga], PB[gbk]])
                P.op("act", lambda e, gbk=gbk, pp=pp, ln=ln: e.activation(out=sgb[pp][:, 0:ln], in_=psf(gbk)[:, 0:ln], func=ACT.Sigmoid), reads=[PB[gbk]], writes=[b_sgb[pp]])
                P.op("dve", lambda e, ga=ga, pp=pp, c=c, st=st, ln=ln: e.tensor_tensor(out=uT[:, c, st:st + ln], in0=psf(ga)[:, 0:ln], in1=sgb[pp][:, 0:ln], op=ALU.mult),
                     reads=[PB[ga], b_sgb[pp]], writes=[b_uT[c][si]])
        P.barrier()
        AR.lo = mark_cv
        wstate["ptr"] = w_after_diag
        vv2 = [W([128, 4, 512], F32)]
        sqv2 = [W([128, 4, 512], F32)]
        var = W([128, 512], F32)
        rsd = W([128, 512], F32)
        mean = AR.left([128, 512], F32)
        dd = [AR.left([128, 512], F32) for _ in range(3)]
        vv2.append(AR.left([128, 4, 512], F32))
        sqv2.append(AR.left([128, 4, 512], F32))
        b_vv2 = [[Buf() for _ in range(4)] for _ in range(2)]
        b_sqv2 = [[Buf() for _ in range(4)] for _ in range(2)]
        b_dd = [Buf(), Buf(), Buf()]
        m2 = dd[2]
        lv = var
        b_st = Buf("lnstats")

        def stageC(n):
            z = n % 2
            vv, sqv, b_vv, b_sqv = vv2[z], sqv2[z], b_vv2[z], b_sqv2[z]
            for c in range(4):
                cvb = 4 + (c % 2)

                def mmconv(e, c=c, cvb=cvb):
                    ins = None
                    for j in range(31):
                        s0 = 128 + n * 512 - 30 + j
                        ins = e.matmul(out=psf(cvb), lhsT=diag[:, c, j, :], rhs=uT[:, c, s0:s0 + 512], start=(j == 0), stop=(j == 30))
                    return ins
                P.op("pe", mmconv, reads=[b_diag[c], b_uT[c][n], b_uT[c][n + 1]], writes=[PB[cvb]])
                P.op("act", lambda e, c=c, cvb=cvb: e.activation(out=vv[:, c, :], in_=psf(cvb), func=ACT.Identity, bias=cp[:, BDW + c:BDW + c + 1], scale=1.0),
                     reads=[PB[cvb], b_cp], writes=[b_vv[c]])
                P.op("pool", lambda e, c=c: e.tensor_tensor(out=sqv[:, c, :], in0=vv[:, c, :], in1=vv[:, c, :], op=ALU.mult), reads=[b_vv[c]], writes=[b_sqv[c]])

            sa, sq_ = (6, 7) if z == 0 else (2, 3)

            def mmst(e):
                ins = None
                for c in range(4):
                    ins = e.matmul(out=psf(sa), lhsT=ones_f, rhs=vv[:, c, :], start=(c == 0), stop=(c == 3))
                for c in range(4):
                    ins = e.matmul(out=psf(sq_), lhsT=ones_f, rhs=sqv[:, c, :], start=(c == 0), stop=(c == 3))
                return ins
            P.op("pe", mmst, reads=b_vv + b_sqv + [b_const], writes=[PB[sa], PB[sq_]])

        def stageL(n):
            z = n % 2
            vv, b_vv = vv2[z], b_vv2[z]
            sa, sq_ = (6, 7) if z == 0 else (2, 3)
            P.op("dve", lambda e: e.tensor_scalar(out=mean, in0=psf(sa), scalar1=1.0 / 512, scalar2=None, op0=ALU.mult), reads=[PB[sa]], writes=[b_st])
            P.op("dve", lambda e: e.tensor_tensor(out=m2, in0=mean, in1=mean, op=ALU.mult), reads=[b_st], writes=[b_st])
            P.op("dve", lambda e: e.scalar_tensor_tensor(out=var, in0=psf(sq_), scalar=1.0 / 512, in1=m2, op0=ALU.mult, op1=ALU.subtract), reads=[PB[sq_], b_st], writes=[b_st])
            P.op("act", lambda e: e.activation(out=lv, in_=var, func=ACT.Ln, bias=eps5[:, 0:1], scale=1.0), reads=[b_st, b_const], writes=[b_st])
            P.op("act", lambda e: e.activation(out=rsd, in_=lv, func=ACT.Exp, scale=-0.5), reads=[b_st], writes=[b_st])
            for c in range(4):
                pp = c % 2
                P.op("dve", lambda e, c=c, pp=pp: e.tensor_tensor(out=dd[pp], in0=vv[:, c, :], in1=mean, op=ALU.subtract), reads=[b_vv[c], b_st], writes=[b_dd[pp]])
                P.op("dve", lambda e, pp=pp: e.tensor_tensor(out=dd[pp], in0=dd[pp], in1=rsd, op=ALU.mult), reads=[b_st], writes=[b_dd[pp]])
                P.op("act", lambda e, c=c, pp=pp: e.activation(out=bT[:, c, n * 512:(n + 1) * 512], in_=dd[pp], func=ACT.Silu,
                                                               scale=cp[:, GLN + c:GLN + c + 1], bias=cp[:, BLN + c:BLN + c + 1]),
                     reads=[b_dd[pp], b_cp], writes=b_bT[n * 4:(n + 1) * 4])

        stageC(0)
        for n in range(NS):
            if n + 1 < NS:
                stageC(n + 1)
            stageL(n)
        P.barrier()
        maybe_stop("conv")
        AR.lo = mark1
        branch_merge(wco_d, OFF_GATE + 1024, False)
        P.barrier()
        maybe_stop("conv_merge")
        AR.lo = mark1

        Wreset()
        wC = W([128, 8, 512], BF16)
        b_wC = Buf("wC")
        s_wC = P.dma_sem("wC")
        xqT = W([128, 4, NT], BF16)
        b_xqT = [Buf() for _ in range(TT)]
        sqx = [W([128, 512], F32) for _ in range(2)]
        t1x = [W([128, 512], F32) for _ in range(2)]
        xqn = [W([128, 512], BF16) for _ in range(2)]
        ss4 = [W([128, 4], F32) for _ in range(2)]
        l4 = [W([128, 4], F32) for _ in range(2)]
        r4 = [W([128, 4], F32) for _ in range(2)]
        Pmx = [W([128, 512], BF16) for _ in range(4)]
        b_Pmx = [Buf() for _ in range(4)]
        rdx = [W([128, 512], F32) for _ in range(2)]
        b_rdx = [Buf(), Buf()]
        b_x = [Buf(), Buf()]
        b_xqn = [Buf(), Buf()]
        cast_dma(wC, w_in_d[:, OFF_XQ:OFF_XQ + 512].rearrange("(kc p) n -> p kc n", p=128), b_wC, s_wC)

        def stageQ1(t):
            z = t % 2
            xb = t % 2

            def mmxq(e):
                ins = None
                for kc in range(8):
                    ins = e.matmul(out=psf(xb), lhsT=hT[:, kc, (t + 1) * 128:(t + 2) * 128], rhs=wC[:, kc, :], start=(kc == 0), stop=(kc == 7))
                return ins
            P.op("pe", mmxq, reads=[b_hT[t + 1], b_wC], writes=[PB[xb]])
            P.op("act", lambda e: e.activation(out=sqx[z], in_=psf(xb), func=ACT.Square), reads=[PB[xb]], writes=[b_x[z]])
            P.op("dve", lambda e: e.tensor_reduce(out=ss4[z], in_=sqx[z].rearrange("p (h d) -> p h d", h=4), axis=AX.X, op=ALU.add), reads=[b_x[z]], writes=[b_x[z]])
            P.op("act", lambda e: e.activation(out=l4[z], in_=ss4[z], func=ACT.Ln, scale=1.0 / 128, bias=eps6[:, 0:1]), reads=[b_x[z], b_const], writes=[b_x[z]])
            P.op("act", lambda e: e.activation(out=r4[z], in_=l4[z], func=ACT.Exp, scale=-0.5), reads=[b_x[z]], writes=[b_x[z]])
            P.op("dve", lambda e: e.tensor_tensor(out=t1x[z].rearrange("p (h d) -> p h d", h=4), in0=psf(xb).rearrange("p (h d) -> p h d", h=4),
                                                  in1=r4[z].unsqueeze(2).to_broadcast([128, 4, 128]), op=ALU.mult), reads=[PB[xb], b_x[z]], writes=[b_x[z]])
            P.op("pool", lambda e: e.tensor_tensor(out=xqn[z], in0=t1x[z], in1=cp[:, GXQ:GXQ + 512], op=ALU.mult), reads=[b_x[z], b_cp], writes=[b_xqn[z]])

        def stageQ2(t):
            z = t % 2

            def trx(e):
                ins = None
                for h in range(4):
                    ins = e.transpose(out=psb(2)[:, h * 128:(h + 1) * 128], in_=xqn[z][:, h * 128:(h + 1) * 128], identity=identb)
                return ins
            P.op("pe", trx, reads=[b_xqn[z], b_const], writes=[PB[2]])
            P.op("act", lambda e: e.activation(out=xqT[:, :, t * 128:(t + 1) * 128], in_=psb(2)[:, 0:512].rearrange("p (h t) -> p h t", h=4), func=ACT.Copy),
                 reads=[PB[2]], writes=[b_xqT[t]])

        stageQ1(0)
        for t in range(TT):
            if t + 1 < TT:
                stageQ1(t + 1)
            stageQ2(t)
        it = 0
        for n in range(NS):
            for h in range(4):
                pp = it % 2
                it += 1
                for mt in range(2):
                    sbk = 3 + mt
                    pm = 2 * pp + mt
                    P.op("pe", lambda e, h=h, n=n, mt=mt, sbk=sbk: e.matmul(out=psf(sbk), lhsT=mkT[:, h, mt * 128:(mt + 1) * 128],
                                                                            rhs=xqT[:, h, n * 512:(n + 1) * 512], start=True, stop=True),
                         reads=[b_mkT] + b_xqT[n * 4:(n + 1) * 4], writes=[PB[sbk]])
                    P.op("act", lambda e, sbk=sbk, pm=pm: e.activation(out=Pmx[pm], in_=psf(sbk), func=ACT.Exp, scale=128.0 ** -0.5), reads=[PB[sbk]], writes=[b_Pmx[pm]])
                ob, db = 5, 6 + pp

                def mmo(e, h=h, pp=pp, ob=ob, db=db):
                    ins = None
                    for mt in range(2):
                        ins = e.matmul(out=psf(ob), lhsT=mv_sb[:, mt, h * 128:(h + 1) * 128], rhs=Pmx[2 * pp + mt], start=(mt == 0), stop=(mt == 1))
                    for mt in range(2):
                        ins = e.matmul(out=psf(db), lhsT=ones_b, rhs=Pmx[2 * pp + mt], start=(mt == 0), stop=(mt == 1))
                    return ins
                P.op("pe", mmo, reads=[b_mv, b_const, b_Pmx[2 * pp], b_Pmx[2 * pp + 1]], writes=[PB[ob], PB[db]])
                P.op("dve", lambda e, pp=pp, db=db: e.reciprocal(out=rdx[pp], in_=psf(db)), reads=[PB[db]], writes=[b_rdx[pp]])
                P.op("dve", lambda e, pp=pp, ob=ob, h=h, n=n: e.tensor_tensor(out=bT[:, h, n * 512:(n + 1) * 512], in0=psf(ob), in1=rdx[pp], op=ALU.mult),
                     reads=[PB[ob], b_rdx[pp]], writes=b_bT[n * 4:(n + 1) * 4])
        P.barrier()
        maybe_stop("mem")
        AR.lo = mark1
        branch_merge(wom_d, OFF_GATE + 2048, False)
        P.barrier()
        maybe_stop("mem_merge")
        if debug:
            s_dbg = P.dma_sem("dbg")
            b_dbg = Buf("dbg")
            P.op("sp", lambda e: e.dma_start(out=dbg["merged"], in_=merged.rearrange("p a b -> p (a b)")), writes=[b_dbg], dma=s_dbg)
            P.op("sp", lambda e: e.dma_start(out=dbg["hT"], in_=hT.rearrange("p a b -> p (a b)")), writes=[b_dbg], dma=s_dbg)
            P.barrier()

        AR.lo = mark0 + ((8 * NT * 2 + 63) // 64 * 64)
        x1acc = AR.right([128, TT, D], F32)
        h2T = AR.right([128, 8, NT], BF16)
        b_x1 = [[Buf(), Buf()] for _ in range(TT)]
        b_h2T = [Buf() for _ in range(TT)]
        Wreset()
        wout = W([128, 8, D], BF16)
        b_wout = Buf("wout")
        s_wout = P.dma_sem("wout")
        wg = [None, None]
        wu = [None, None]
        wd = [None, None]
        wg[0] = W([128, 8, FF], BF16)
        wu[0] = W([128, 8, FF], BF16)
        wd[0] = W([128, 4, D], BF16)
        wd[1] = W([128, 4, D], BF16)
        _save_ptr = wstate["ptr"]
        wstate["ptr"] = wstate["base"]
        wg[1] = W([128, 8, FF], BF16)
        wu[1] = W([128, 8, FF], BF16)
        wstate["ptr"] = _save_ptr
        b_wg = [Buf(), Buf()]
        b_wu = [Buf(), Buf()]
        b_wd = [Buf(), Buf()]
        s_wg = [P.dma_sem("wg0"), P.dma_sem("wg1")]
        s_wu = [P.dma_sem("wu0"), P.dma_sem("wu1")]
        s_wd = [P.dma_sem("wd0"), P.dma_sem("wd1")]

        def issue_expert(ex):
            b = ex % 2
            extra = [b_wout] if b == 1 else []
            P.op("pool", lambda e: e.dma_start(out=wg[b], in_=wg_d[ex].rearrange("(kc p) n -> p kc n", p=128)), writes=[b_wg[b]] + extra, dma=s_wg[b])
            P.op("pool", lambda e: e.dma_start(out=wu[b], in_=wu_d[ex].rearrange("(kc p) n -> p kc n", p=128)), writes=[b_wu[b]] + extra, dma=s_wu[b])
            P.op("pool", lambda e: e.dma_start(out=wd[b], in_=wd_d[ex].rearrange("(kc p) n -> p kc n", p=128)), writes=[b_wd[b]], dma=s_wd[b])
        wgr = AR.left([128, 8, 20], F32)
        b_wgr = Buf("wgr")
        s_wgr = P.dma_sem("wgr")
        xt2 = [AR.left([128, D], F32) for _ in range(2)]
        b_xt2 = [Buf(), Buf()]
        s_xt2 = [P.dma_sem("xt2a"), P.dma_sem("xt2b")]
        junk2 = AR.left([128, D], BF16)
        b_junk2 = Buf()
        ss2 = AR.left([128, TT], F32)
        l2 = AR.left([128, TT], F32)
        r2 = AR.left([128, TT], F32)
        b_r2 = [Buf() for _ in range(TT)]
        h2 = [AR.left([128, D], F32) for _ in range(2)]
        b_h2 = [Buf(), Buf()]
        h2Tf = [AR.left([128, 8, 128], F32) for _ in range(2)]
        b_h2Tf = [Buf(), Buf()]
        lg_all = AR.left([128, TT, 20], F32)
        b_lg = Buf("lg")
        cast_dma(wout, wout_d.rearrange("(kc p) n -> p kc n", p=128), b_wout, s_wout)
        if n_experts > 0:
            issue_expert(0)
        P.op("sp", lambda e: e.dma_start(out=wgr, in_=wgr_d.rearrange("(kc p) n -> p kc n", p=128)), writes=[b_wgr], dma=s_wgr)
        g2b = cp[:, G2C:G2C + 8].unsqueeze(2).to_broadcast([128, 8, 128])
        brt_b = cp[:, BRT:BRT + 20]
        def stageX(t):
            k = t % 2
            P.op("sp", lambda e, k=k, t=t: e.dma_start(out=xt2[k], in_=x_d[pi * NT + t * 128:pi * NT + (t + 1) * 128, :]), writes=[b_xt2[k]], dma=s_xt2[k])
            for half in range(2):
                xb = 0 + half

                def mmx1(e, t=t, half=half, xb=xb):
                    ins = None
                    for m in range(8):
                        ins = e.matmul(out=psf(xb), lhsT=merged[:, m, t * 128:(t + 1) * 128], rhs=wout[:, m, half * 512:(half + 1) * 512],
                                       start=(m == 0), stop=(m == 7))
                    return ins
                P.op("pe", mmx1, reads=[b_wout] + [b_merged[m][t // 4] for m in range(8)], writes=[PB[xb]])
                P.op("dve", lambda e, t=t, half=half, xb=xb, k=k: e.tensor_tensor(out=x1acc[:, t, half * 512:(half + 1) * 512], in0=psf(xb),
                                                                                 in1=xt2[k][:, half * 512:(half + 1) * 512], op=ALU.add),
                     reads=[PB[xb], b_xt2[k]], writes=[b_x1[t][half]])
            P.op("act", lambda e, t=t: e.activation(out=junk2, in_=x1acc[:, t, :], func=ACT.Square, accum_out=ss2[:, t:t + 1]),
                 reads=b_x1[t], writes=[b_junk2, b_r2[t]])
            P.op("act", lambda e, t=t: e.activation(out=l2[:, t:t + 1], in_=ss2[:, t:t + 1], func=ACT.Ln, scale=1.0 / D, bias=eps6[:, 0:1]), reads=[b_const], writes=[b_r2[t]])
            P.op("act", lambda e, t=t: e.activation(out=r2[:, t:t + 1], in_=l2[:, t:t + 1], func=ACT.Exp, scale=-0.5), writes=[b_r2[t]])
            P.op("dve", lambda e, t=t, k=k: e.tensor_scalar(out=h2[k], in0=x1acc[:, t, :], scalar1=r2[:, t:t + 1], scalar2=None, op0=ALU.mult),
                 reads=b_x1[t] + [b_r2[t]], writes=[b_h2[k]])


        def stageT(t):
            k = t % 2
            def trh(e, k=k):
                ins = None
                for c in range(8):
                    ins = e.transpose(out=psf(2 + c // 4)[:, (c % 4) * 128:(c % 4 + 1) * 128], in_=h2[k][:, c * 128:(c + 1) * 128], identity=identf)
                return ins
            P.op("pe", trh, reads=[b_h2[k], b_cp], writes=[PB[2], PB[3]])
            for hh in range(2):
                P.op("dve", lambda e, k=k, hh=hh: e.tensor_tensor(out=h2Tf[k][:, hh * 4:(hh + 1) * 4, :], in0=psf(2 + hh).rearrange("p (c t) -> p c t", c=4),
                                                                  in1=g2b[:, hh * 4:(hh + 1) * 4, :], op=ALU.mult),
                     reads=[PB[2 + hh], b_cp], writes=[b_h2Tf[k]])
            P.op("pool", lambda e, k=k, t=t: e.tensor_copy(out=h2T[:, :, t * 128:(t + 1) * 128], in_=h2Tf[k]), reads=[b_h2Tf[k]], writes=[b_h2T[t]])

            def mmlg(e, k=k):
                ins = None
                for kc in range(8):
                    ins = e.matmul(out=psf(4)[:, 0:20], lhsT=h2Tf[k][:, kc, :], rhs=wgr[:, kc, :], start=(kc == 0), stop=(kc == 7))
                return ins
            P.op("pe", mmlg, reads=[b_h2Tf[k], b_wgr], writes=[PB[4]])
            P.op("dve", lambda e, t=t: e.tensor_tensor(out=lg_all[:, t, :], in0=psf(4)[:, 0:20], in1=brt_b, op=ALU.add), reads=[PB[4], b_cp], writes=[b_lg])

        stageX(0)
        for t in range(TT):
            if t + 1 < TT:
                stageX(t + 1)
            stageT(t)

        def sm(shape):
            return AR.left(shape, F32)
        gl = lg_all[:, :, 0:4]
        el = lg_all[:, :, 4:20].rearrange("p t (g e) -> p t g e", g=4)
        gmax = sm([128, TT])
        ohg = sm([128, TT, 4])
        gd = sm([128, TT, 4])
        gex = sm([128, TT, 4])
        gsum = sm([128, TT])
        pg = sm([128, TT])
        elm = sm([128, TT, 4, 4])
        els = sm([128, TT, 4])
        m1 = sm([128, TT])
        oh1 = sm([128, TT, 4])
        els2 = sm([128, TT, 4])
        m2r = sm([128, TT])
        oh2 = sm([128, TT, 4])
        ddm = sm([128, TT])
        ee = sm([128, TT])
        s1 = sm([128, TT])
        p1 = sm([128, TT])
        p2 = sm([128, TT])
        wa = sm([128, TT, 4])
        wb2 = sm([128, TT, 4])
        wsel = sm([128, TT, 4])
        b_rt = Buf("route")

        def R(eng, fn, extra_r=(), w=None):
            P.op(eng, fn, reads=[b_rt, b_lg] + list(extra_r), writes=[b_rt] if w is None else w)

        def bc3(a):
            return a.unsqueeze(2).to_broadcast([128, TT, 4])
        R("dve", lambda e: e.tensor_reduce(out=gmax, in_=gl, axis=AX.X, op=ALU.max))
        R("dve", lambda e: e.tensor_tensor(out=ohg, in0=gl, in1=bc3(gmax), op=ALU.is_equal))
        R("dve", lambda e: e.tensor_tensor(out=gd, in0=gl, in1=bc3(gmax), op=ALU.subtract))
        R("act", lambda e: e.activation(out=gex, in_=gd, func=ACT.Exp))
        R("dve", lambda e: e.tensor_reduce(out=gsum, in_=gex, axis=AX.X, op=ALU.add))
        R("dve", lambda e: e.reciprocal(out=pg, in_=gsum))
        R("dve", lambda e: e.tensor_tensor(out=elm, in0=el, in1=ohg.unsqueeze(3).to_broadcast([128, TT, 4, 4]), op=ALU.mult))
        R("dve", lambda e: e.tensor_reduce(out=els, in_=elm.rearrange("p t g e -> p t e g"), axis=AX.X, op=ALU.add))
        R("dve", lambda e: e.tensor_reduce(out=m1, in_=els, axis=AX.X, op=ALU.max))
        R("dve", lambda e: e.tensor_tensor(out=oh1, in0=els, in1=bc3(m1), op=ALU.is_equal))
        R("dve", lambda e: e.scalar_tensor_tensor(out=els2, in0=oh1, scalar=-1e30, in1=els, op0=ALU.mult, op1=ALU.add))
        R("dve", lambda e: e.tensor_reduce(out=m2r, in_=els2, axis=AX.X, op=ALU.max))
        R("dve", lambda e: e.tensor_tensor(out=oh2, in0=els2, in1=bc3(m2r), op=ALU.is_equal))
        R("dve", lambda e: e.tensor_tensor(out=ddm, in0=m2r, in1=m1, op=ALU.subtract))
        R("act", lambda e: e.activation(out=ee, in_=ddm, func=ACT.Exp))
        R("dve", lambda e: e.tensor_scalar(out=s1, in0=ee, scalar1=1.0, scalar2=None, op0=ALU.add))
        R("dve", lambda e: e.reciprocal(out=p1, in_=s1))
        R("dve", lambda e: e.tensor_tensor(out=p2, in0=ee, in1=p1, op=ALU.mult))
        R("dve", lambda e: e.tensor_tensor(out=p1, in0=p1, in1=pg, op=ALU.mult))
        R("dve", lambda e: e.tensor_tensor(out=p2, in0=p2, in1=pg, op=ALU.mult))
        R("dve", lambda e: e.tensor_tensor(out=wa, in0=oh1, in1=bc3(p1), op=ALU.mult))
        R("dve", lambda e: e.tensor_tensor(out=wb2, in0=oh2, in1=bc3(p2), op=ALU.mult))
        R("dve", lambda e: e.tensor_tensor(out=wsel, in0=wa, in1=wb2, op=ALU.add))
        R("dve", lambda e: e.tensor_tensor(out=c_all.rearrange("p t (g e) -> p t g e", g=4), in0=ohg.unsqueeze(3).to_broadcast([128, TT, 4, 4]),
                                           in1=wsel.unsqueeze(2).to_broadcast([128, TT, 4, 4]), op=ALU.mult), w=[b_rt, b_call])
        if debug:
            P.barrier()
        if debug:
            for t in range(TT):
                P.op("sp", lambda e, t=t: e.dma_start(out=dbg["x1"][t * 128:(t + 1) * 128, :], in_=x1acc[:, t, :]), writes=[b_dbg], dma=s_dbg)
            P.op("sp", lambda e: e.dma_start(out=dbg["call"], in_=c_all.rearrange("p t e -> p (t e)")), writes=[b_dbg], dma=s_dbg)
            P.barrier()

        AR.lo = mark0
        hid = AR.left([128, 4, NT], BF16)
        b_hid = [[Buf() for _ in range(NS)] for _ in range(4)]
        sgm = [AR.left([128, 512], F32) for _ in range(2)]
        b_sgm = [Buf(), Buf()]
        it = 0
        iy = 0
        for ex in range(n_experts):
            b = ex % 2
            if ex > 0:
                issue_expert(ex)
            for n in range(NS):
                for f in range(4):
                    pp = it % 2
                    it += 1
                    gp, up = 0 + pp, 2 + pp

                    def mmgu(e, b=b, n=n, f=f, gp=gp, up=up):
                        ins = None
                        for kc in range(8):
                            ins = e.matmul(out=psf(gp), lhsT=wg[b][:, kc, f * 128:(f + 1) * 128], rhs=h2T[:, kc, n * 512:(n + 1) * 512], start=(kc == 0), stop=(kc == 7))
                        for kc in range(8):
                            ins = e.matmul(out=psf(up), lhsT=wu[b][:, kc, f * 128:(f + 1) * 128], rhs=h2T[:, kc, n * 512:(n + 1) * 512], start=(kc == 0), stop=(kc == 7))
                        return ins
                    P.op("pe", mmgu, reads=[b_wg[b], b_wu[b]] + b_h2T[n * 4:(n + 1) * 4], writes=[PB[gp], PB[up]])
                    P.op("act", lambda e, gp=gp, pp=pp: e.activation(out=sgm[pp], in_=psf(gp), func=ACT.Silu), reads=[PB[gp]], writes=[b_sgm[pp]])
                    P.op("dve", lambda e, up=up, pp=pp, f=f, n=n: e.tensor_tensor(out=hid[:, f, n * 512:(n + 1) * 512], in0=psf(up), in1=sgm[pp], op=ALU.mult),
                         reads=[PB[up], b_sgm[pp]], writes=[b_hid[f][n]])
            for t in range(TT):
                for half in range(2):
                    yb = 4 + iy % 4
                    iy += 1

                    def mmd(e, b=b, t=t, half=half, yb=yb):
                        ins = None
                        for f in range(4):
                            ins = e.matmul(out=psf(yb), lhsT=hid[:, f, t * 128:(t + 1) * 128], rhs=wd[b][:, f, half * 512:(half + 1) * 512], start=(f == 0), stop=(f == 3))
                        return ins
                    P.op("pe", mmd, reads=[b_wd[b]] + [b_hid[f][t // 4] for f in range(4)], writes=[PB[yb]])
                    dst = x1acc[:, t, half * 512:(half + 1) * 512]
                    P.op("dve", lambda e, yb=yb, t=t, ex=ex, dst=dst: e.scalar_tensor_tensor(out=dst, in0=psf(yb), scalar=c_all[:, t, ex:ex + 1], in1=dst,
                                                                                             op0=ALU.mult, op1=ALU.add),
                         reads=[PB[yb], b_call], writes=[b_x1[t][half]])
        s_out = [P.dma_sem("out%d" % i) for i in range(2)]
        b_out = [Buf(), Buf()]
        for t in range(TT):
            P.op("sp", lambda e, t=t: e.dma_start(out=out_d[pi * NT + t * 128:pi * NT + (t + 1) * 128, :], in_=x1acc[:, t, :]), reads=b_x1[t], writes=[b_out[t % 2]], dma=s_out[t % 2])
        P.barrier()

    for pi in (range(NPASS) if passes is None else passes):
        AR.lo = mark0
        AR.hi = arena_hi0
        run_pass(pi)
    P.barrier()
    P.emit()
    return nc


def make_cp(inp, first):
    f32 = np.float32
    cp = np.zeros((128, NCP), f32)
    cp[:, G1C:G1C + 8] = inp["g_norm1"][0].reshape(8, 128).T
    cp[:, G2C:G2C + 8] = inp["g_norm2"][0].reshape(8, 128).T
    cp[:, GMC:GMC + 8] = inp["g_mem"][0].reshape(8, 128).T
    cp[:, BDW:BDW + 4] = inp["b_conv_dw"][0].reshape(4, 128).T
    cp[:, GLN:GLN + 4] = inp["g_conv_ln"][0].reshape(4, 128).T
    cp[:, BLN:BLN + 4] = inp["b_conv_ln"][0].reshape(4, 128).T
    w = np.asarray(inp["w_conv_dw"][0])
    cp[:, WDW:WDW + 124] = w.T.reshape(4, 128, 31).transpose(1, 0, 2).reshape(128, 124)
    cp[:, GQK:GQK + 512] = np.tile(inp["g_q"][0], 8)[None, :]
    cp[:, GQK + 512:GQK + 640] = np.tile(inp["g_k"][0], 2)[None, :]
    cp[:, GXQ:GXQ + 512] = np.tile(inp["g_xq"][0], 4)[None, :]
    cp[:, GXK:GXK + 512] = np.tile(inp["g_xk"][0], 4)[None, :]
    cp[:, SINK:SINK + 8] = inp["sinks"][0][None, :]
    cp[:, BRT:BRT + 4] = inp["b_group"][0][None, :]
    cp[:, BRT + 4:BRT + 20] = inp["b_router"][0][None, :]
    inv_freq = (1.0 / (np.float32(10000.0) ** (np.arange(0, 64, 2, dtype=f32) / np.float32(64)))).astype(f32)
    cp[:, INVF:INVF + 32] = inv_freq[None, :]
    cp[:, IDENT:IDENT + 128] = np.eye(128, dtype=f32)
    kk = np.arange(128)[:, None]
    qq = np.arange(128)[None, :]
    cp[:, MASKC:MASKC + 128] = (kk <= qq).astype(f32)
    cp[:, HFLAG] = 0.0 if first else 1.0
    return cp


def make_in_maps(inp, NT, ncores):
    f32 = np.float32
    x = np.asarray(inp["x"], f32)
    B, S, _ = x.shape
    per_b = S // NT
    assert B * per_b == ncores
    pos = np.asarray(inp["positions"], np.int32)
    mem = np.asarray(inp["mem"], f32)
    shared = {
        "w_in": np.ascontiguousarray(inp["w_in"][0], f32),
        "w_o_attn": np.ascontiguousarray(inp["w_o_attn"][0], f32),
        "w_conv_out": np.ascontiguousarray(inp["w_conv_out"][0], f32),
        "w_kv_mem": np.ascontiguousarray(inp["w_kv_mem"][0], f32),
        "w_o_mem": np.ascontiguousarray(inp["w_o_mem"][0], f32),
        "w_out": np.ascontiguousarray(inp["w_out"][0], f32),
        "w_gr": np.ascontiguousarray(np.concatenate([inp["w_group"][0], inp["w_router"][0]], axis=1), f32),
        "w_gate": np.ascontiguousarray(inp["w_gate"][0], f32),
        "w_up": np.ascontiguousarray(inp["w_up"][0], f32),
        "w_down": np.ascontiguousarray(inp["w_down"][0], f32),
    }
    cps = {True: make_cp(inp, True), False: make_cp(inp, False)}
    maps = []
    for c in range(ncores):
        b, j = divmod(c, per_b)
        s0 = j * NT
        first = (j == 0)
        xh = np.zeros((128, D), f32) if first else x[b, s0 - 128:s0]
        ph = np.zeros((128,), np.int32) if first else pos[b, s0 - 128:s0]
        pall = np.concatenate([ph, pos[b, s0:s0 + NT]]).reshape(NT // 128 + 1, 128).T
        m = {"x": np.ascontiguousarray(x[b, s0:s0 + NT]), "xh": np.ascontiguousarray(xh), "pos": np.ascontiguousarray(pall, np.int32),
             "mem": np.ascontiguousarray(mem[b]), "cp": cps[first]}
        m.update(shared)
        maps.append(m)
    return maps


_NC_CACHE = {}


def kernel(**inputs):
    inp = {k: np.asarray(v) for k, v in inputs.items()}
    B, S, _ = inp["x"].shape
    NT = B * S // NCORES
    if NT not in _NC_CACHE:
        _NC_CACHE[NT] = build_nc(NT)
    nc = _NC_CACHE[NT]
    maps = make_in_maps(inp, NT, NCORES)
    res = run_bass_kernel_spmd(nc, maps, core_ids=list(range(NCORES)))
    out = np.stack([r["out"] for r in res.results], axis=0)
    return out.reshape(B, S, D).astype(np.float32)
```

```python
import math
import numpy as np
import concourse.bass as bass
import concourse.mybir as mybir
from concourse.bass_utils import run_bass_kernel_spmd

F32 = mybir.dt.float32
BF16 = mybir.dt.bfloat16
I32 = mybir.dt.int32
ACT = mybir.ActivationFunctionType
ALU = mybir.AluOpType
AX = mybir.AxisListType

D = 1024
NCORES = 8
MEM_LEN = 256
NE = 16
FF = 512
OFF_Q, OFF_K, OFF_V, OFF_GLU, OFF_XQ, OFF_GATE = 0, 512, 640, 768, 1792, 2304
IN_W = 5376

G1C, G2C, GMC, BDW, GLN, BLN, WDW = 0, 8, 16, 24, 28, 32, 36
GQK, GXQ, GXK, SINK, BRT, INVF, IDENT, MASKC, HFLAG = 160, 800, 1312, 1824, 1832, 1852, 1884, 2012, 2140
NCP = 2144

ENGS = ("sp", "act", "pool", "dve", "pe")
SAME_ENGINE_SYNC = True


class Buf:
    __slots__ = ("name", "last_write", "reads")

    def __init__(self, name=""):
        self.name = name
        self.last_write = None
        self.reads = {}


class Prog:
    def __init__(self, nc, same_engine_sync=True):
        self.nc = nc
        self.same_engine_sync = same_engine_sync
        self.ops = {e: [] for e in ENGS}
        self.sem = {}
        self.cnt = {}
        for e in ENGS:
            self.sem[e] = nc.alloc_semaphore("c_" + e)
            self.cnt[e] = 0
        self.waited = {e: {} for e in ENGS}
        self.n_dma_sems = 0
        self.disabled = False

    def dma_sem(self, name="d"):
        key = "dma_%s_%d" % (name, self.n_dma_sems)
        self.n_dma_sems += 1
        self.sem[key] = self.nc.alloc_semaphore(key)
        self.cnt[key] = 0
        return key

    def _collect(self, reads, writes):
        need = {}

        def add(k, v):
            if v > need.get(k, 0):
                need[k] = v
        for b in reads:
            if b.last_write is not None:
                add(*b.last_write)
        for b in writes:
            if b.last_write is not None:
                add(*b.last_write)
            for k, v in b.reads.items():
                add(k, v)
        return need

    def op(self, eng, fn, reads=(), writes=(), dma=None):
        if self.disabled:
            return 0
        need = self._collect(reads, writes)
        waits = []
        for k, v in need.items():
            if k == eng and dma is None:
                if eng == "pe" or not self.same_engine_sync:
                    continue
            if v > self.waited[eng].get(k, 0):
                waits.append((k, v))
                self.waited[eng][k] = v
        key = dma if dma is not None else eng
        inc = 16 if dma is not None else 1
        self.cnt[key] += inc
        val = self.cnt[key]
        self.ops[eng].append((waits, fn, key, inc))
        for b in writes:
            b.last_write = (key, val)
            b.reads = {}
        for b in reads:
            if b not in writes:
                if val > b.reads.get(key, 0):
                    b.reads[key] = val
        return val

    def barrier(self):
        if self.disabled:
            return
        for e in ENGS:
            waits = []
            for k, v in self.cnt.items():
                if k == e and v == 0:
                    continue
                if v > self.waited[e].get(k, 0):
                    waits.append((k, v))
                    self.waited[e][k] = v
            if waits:
                self.ops[e].append((waits, None, None, 0))

    def emit(self):
        nc = self.nc
        handles = {"sp": "sync", "act": "scalar", "pool": "gpsimd", "dve": "vector", "pe": "tensor"}
        with nc.Block() as block:
            for e in ENGS:
                ops = self.ops[e]

                def body(engh, ops=ops):
                    for waits, fn, key, inc in ops:
                        for k, v in waits:
                            engh.wait_ge(self.sem[k], v)
                        if fn is not None:
                            ins = fn(engh)
                            ins.then_inc(self.sem[key], inc)
                getattr(block, handles[e])(body)


def _dsize(dt):
    return {F32: 4, BF16: 2, I32: 4}[dt]


class Arena:
    def __init__(self, nc, nbytes):
        self.t = nc.alloc_sbuf_tensor("arena", [128, nbytes // 2], BF16)
        self.lo = 0
        self.hi = nbytes
        self.log = []

    def _view(self, off, shape, dt):
        n = 1
        for s in shape[1:]:
            n *= s
        size = n * _dsize(dt)
        ap = self.t[:, off // 2:(off + size) // 2]
        if dt != BF16:
            ap = ap.bitcast(dt)
        if len(shape) == 3:
            ap = ap.rearrange("p (a b) -> p a b", a=shape[1])
        elif len(shape) == 4:
            ap = ap.rearrange("p (a b c) -> p a b c", a=shape[1], b=shape[2])
        return ap

    def left(self, shape, dt):
        n = 1
        for s in shape[1:]:
            n *= s
        size = (n * _dsize(dt) + 63) // 64 * 64
        off = self.lo
        self.lo += size
        assert self.lo <= self.hi, "SBUF arena overflow (%d > %d)" % (self.lo, self.hi)
        self.log.append((off, size, tuple(shape)))
        return self._view(off, shape, dt)

    def right(self, shape, dt):
        n = 1
        for s in shape[1:]:
            n *= s
        size = (n * _dsize(dt) + 63) // 64 * 64
        self.hi -= size
        assert self.lo <= self.hi, "SBUF arena overflow"
        return self._view(self.hi, shape, dt)


def build_nc(NT=2048, debug=False, n_experts=NE, stop_after=None, NTP=1024, passes=None):
    NTP = min(NTP, NT)
    NPASS = NT // NTP
    NT_FULL = NT
    TT_FULL = NT // 128
    NT = NTP
    TT = NT // 128
    TT1 = TT + 1
    NS = NT // 512
    NTH = NT + 128
    nc = bass.Bass("TRN2", target_bir_lowering=False)

    def din(name, shape, dt=F32):
        return nc.dram_tensor(name, list(shape), dt, kind="ExternalInput").ap()

    x_d = din("x", [NT_FULL, D])
    xh_d = din("xh", [128, D])
    pos_d = din("pos", [128, TT_FULL + 1], I32)
    mem_d = din("mem", [MEM_LEN, D])
    cp_d = din("cp", [128, NCP])
    w_in_d = din("w_in", [D, IN_W])
    woa_d = din("w_o_attn", [512, D])
    wco_d = din("w_conv_out", [512, D])
    wkv_d = din("w_kv_mem", [D, 1024])
    wom_d = din("w_o_mem", [512, D])
    wout_d = din("w_out", [D, D])
    wgr_d = din("w_gr", [D, 20])
    wg_d = din("w_gate", [NE, D, FF])
    wu_d = din("w_up", [NE, D, FF])
    wd_d = din("w_down", [NE, FF, D])
    out_d = nc.dram_tensor("out", [NT_FULL, D], F32, kind="ExternalOutput").ap()
    dbg = {}
    if debug:
        dbg["x1"] = nc.dram_tensor("d_x1", [NT, D], F32, kind="ExternalOutput").ap()
        dbg["call"] = nc.dram_tensor("d_call", [128, TT * 16], F32, kind="ExternalOutput").ap()
        dbg["merged"] = nc.dram_tensor("d_merged", [128, 8 * NT], BF16, kind="ExternalOutput").ap()
        dbg["hT"] = nc.dram_tensor("d_hT", [128, 8 * NTH], BF16, kind="ExternalOutput").ap()

    P = Prog(nc, same_engine_sync=SAME_ENGINE_SYNC)
    posi_t = nc.alloc_sbuf_tensor("posi", [128, TT1], I32)
    ki_t = nc.alloc_sbuf_tensor("ki", [128, TT1, 32], I32)
    AR = Arena(nc, 229344 - 16512 - 64 - 2048)
    arena_hi0 = AR.hi
    PS = [nc.alloc_psum_tensor("ps%d" % i, [128, 512], F32) for i in range(8)]
    PB = [Buf("ps%d" % i) for i in range(8)]

    def maybe_stop(tag):
        if stop_after == tag and not P.disabled:
            P.barrier()
            if debug:
                sd = P.dma_sem("dbgstop")
                bd = Buf("dbgstop")
                P.op("sp", lambda e: e.dma_start(out=dbg["hT"], in_=hT.rearrange("p a b -> p (a b)")), writes=[bd], dma=sd)
                P.op("sp", lambda e: e.dma_start(out=dbg["merged"], in_=merged.rearrange("p a b -> p (a b)")), writes=[bd], dma=sd)
            P.barrier()
            P.disabled = True

    def psf(i):
        return PS[i][:, :]

    def psb(i):
        return PS[i][:, :].bitcast(BF16)

    cp = AR.left([128, NCP], F32)
    b_cp = Buf("cp")
    identb = AR.left([128, 128], BF16)
    maskc4 = AR.left([128, 4, 128], BF16)
    maskp4 = AR.left([128, 4, 128], BF16)
    ones_f = AR.left([128, 128], F32)
    ones_b = AR.left([128, 128], BF16)
    esink = AR.left([128, 8], F32)
    eps6 = AR.left([128, 1], F32)
    eps5 = AR.left([128, 1], F32)
    cos_t = AR.left([128, TT1, 32], F32)
    sin_t = AR.left([128, TT1, 32], F32)
    c_all = AR.left([128, TT, 16], F32)
    b_const = Buf("const")
    b_call = Buf("call")
    identf = cp[:, IDENT:IDENT + 128]

    s_cp = P.dma_sem("cp")
    P.op("sp", lambda e: e.dma_start(out=cp, in_=cp_d), writes=[b_cp], dma=s_cp)
    P.op("dve", lambda e: e.tensor_copy(out=identb, in_=identf), reads=[b_cp], writes=[b_const])
    mk_b = cp[:, MASKC:MASKC + 128].unsqueeze(1).to_broadcast([128, 4, 128])
    P.op("dve", lambda e: e.tensor_scalar(out=maskc4, in0=mk_b, scalar1=30000.0, scalar2=-30000.0, op0=ALU.mult, op1=ALU.add),
         reads=[b_cp], writes=[b_const])
    P.op("dve", lambda e: e.tensor_scalar(out=maskp4, in0=mk_b, scalar1=-30000.0, scalar2=None, op0=ALU.mult),
         reads=[b_cp], writes=[b_const])
    P.op("pool", lambda e: e.memset(ones_f, 1.0), writes=[b_const])
    P.op("pool", lambda e: e.memset(ones_b, 1.0), writes=[b_const])
    P.op("pool", lambda e: e.memset(eps6, 1e-6), writes=[b_const])
    P.op("pool", lambda e: e.memset(eps5, 1e-5), writes=[b_const])
    P.op("act", lambda e: e.activation(out=esink, in_=cp[:, SINK:SINK + 8], func=ACT.Exp), reads=[b_cp], writes=[b_const])

    WSIZE = 53248 + 128
    wstate = {"base": AR.lo, "ptr": AR.lo}
    AR.lo += WSIZE

    def W(shape, dt):
        save = AR.lo
        AR.lo = wstate["ptr"]
        ap = AR.left(shape, dt)
        wstate["ptr"] = AR.lo
        assert wstate["ptr"] <= wstate["base"] + WSIZE, "W region overflow"
        AR.lo = save
        return ap

    def Wreset():
        wstate["ptr"] = wstate["base"]

    mark0 = AR.lo

    def run_pass(pi):
        Wreset()
        wkv = W([128, 8, 1024], BF16)
        b_wkv = Buf("wkv")
        s_wkv = P.dma_sem("wkv")
        wA = W([128, 8, 768], BF16)
        b_wA = Buf("wA")
        s_wA = P.dma_sem("wA")
        P.op("pool", lambda e: e.dma_start(out=wkv, in_=wkv_d.rearrange("(kc p) n -> p kc n", p=128)), writes=[b_wkv], dma=s_wkv)
        P.op("pool", lambda e: e.dma_start(out=wA, in_=w_in_d[:, 0:768].rearrange("(kc p) n -> p kc n", p=128)), writes=[b_wA], dma=s_wA)
        posi = posi_t[:, :]
        posf = W([128, TT1], F32)
        ang = W([128, TT1, 32], F32)
        rr = [W([128, TT1, 32], F32) for _ in range(4)]
        ki = ki_t[:, :, :]
        b_r = Buf("rope")
        s_pos = P.dma_sem("pos")
        P.op("sp", lambda e: e.dma_start(out=posi, in_=pos_d[:, pi * TT:pi * TT + TT1]), writes=[b_r], dma=s_pos)
        P.op("dve", lambda e: e.tensor_copy(out=posf, in_=posi), reads=[b_r], writes=[b_r])
        P.op("dve", lambda e: e.tensor_tensor(out=ang, in0=posf.unsqueeze(2).to_broadcast([128, TT1, 32]),
                                              in1=cp[:, INVF:INVF + 32].unsqueeze(1).to_broadcast([128, TT1, 32]), op=ALU.mult),
             reads=[b_r, b_cp], writes=[b_r])
        TWO_PI = 2.0 * math.pi
        C1 = 6.28125
        C2 = TWO_PI - C1
        P.op("dve", lambda e: e.tensor_scalar(out=rr[0], in0=ang, scalar1=1.0 / TWO_PI, scalar2=None, op0=ALU.mult), reads=[b_r], writes=[b_r])
        P.op("dve", lambda e: e.tensor_copy(out=ki, in_=rr[0]), reads=[b_r], writes=[b_r])
        P.op("dve", lambda e: e.tensor_copy(out=rr[0], in_=ki), reads=[b_r], writes=[b_r])
        P.op("dve", lambda e: e.scalar_tensor_tensor(out=rr[1], in0=rr[0], scalar=-C1, in1=ang, op0=ALU.mult, op1=ALU.add), reads=[b_r], writes=[b_r])
        P.op("dve", lambda e: e.scalar_tensor_tensor(out=rr[2], in0=rr[0], scalar=-C2, in1=rr[1], op0=ALU.mult, op1=ALU.add), reads=[b_r], writes=[b_r])
        PI_T = 3.1415925

        def wrap_sin(dst, shift):
            P.op("dve", lambda e: e.tensor_scalar(out=rr[1], in0=rr[2], scalar1=shift, scalar2=None, op0=ALU.add), reads=[b_r], writes=[b_r])
            P.op("dve", lambda e: e.tensor_scalar(out=rr[0], in0=rr[1], scalar1=PI_T, scalar2=None, op0=ALU.is_gt), reads=[b_r], writes=[b_r])
            P.op("dve", lambda e: e.scalar_tensor_tensor(out=rr[3], in0=rr[0], scalar=-TWO_PI, in1=rr[1], op0=ALU.mult, op1=ALU.add), reads=[b_r], writes=[b_r])
            P.op("dve", lambda e: e.tensor_scalar(out=rr[0], in0=rr[3], scalar1=-PI_T, scalar2=None, op0=ALU.is_lt), reads=[b_r], writes=[b_r])
            P.op("dve", lambda e: e.scalar_tensor_tensor(out=rr[1], in0=rr[0], scalar=TWO_PI, in1=rr[3], op0=ALU.mult, op1=ALU.add), reads=[b_r], writes=[b_r])
            P.op("dve", lambda e: e.tensor_scalar(out=rr[3], in0=rr[1], scalar1=-3.141592, scalar2=3.141592, op0=ALU.max, op1=ALU.min), reads=[b_r], writes=[b_r])
            P.op("act", lambda e: e.activation(out=dst, in_=rr[3], func=ACT.Sin), reads=[b_r], writes=[b_const])

        wrap_sin(sin_t, 0.0)
        wrap_sin(cos_t, math.pi / 2)
        AR.lo = mark0

        merged = AR.left([128, 8, NT], BF16)
        hT = AR.left([128, 8, NTH], BF16)
        bT = AR.left([128, 4, NT], BF16)
        mkT = AR.left([128, 4, MEM_LEN], BF16)
        mv_sb = AR.left([128, 2, 512], BF16)
        rstd_all = AR.left([128, TT1 + 2], F32)
        ss_all = AR.left([128, TT1 + 2], F32)
        b_hT = [Buf("hT%d" % t) for t in range(TT1)]
        b_merged = [[Buf() for _ in range(NS)] for _ in range(8)]
        b_bT = [Buf("bT%d" % t) for t in range(TT)]
        b_wo = Buf("wo")
        s_wo = P.dma_sem("wo")
        b_gw = [Buf("gw0"), Buf("gw1")]
        s_gw = [P.dma_sem("gw0"), P.dma_sem("gw1")]
        b_memT = Buf("memT")
        b_mkT = Buf("mkT")
        b_mv = Buf("mv")
        b_rstd = Buf("rstd")
        b_ss = Buf("ss")
        mark1 = AR.lo

        memT = AR.left([128, 8, MEM_LEN], BF16)
        mark1b = AR.lo
        xt = [AR.left([128, D], F32) for _ in range(3)]
        b_xt = [Buf("xt%d" % i) for i in range(3)]
        s_xt = [P.dma_sem("xt%d" % i) for i in range(3)]
        junk = AR.left([128, D], BF16)
        b_junk = Buf("junk")
        xn = [AR.left([128, D], BF16) for _ in range(2)]
        b_xn = [Buf("xn0"), Buf("xn1")]
        lnt = AR.left([128, TT1 + 2], F32)
        NTILES = TT1 + 2

        def src_tile(i):
            if i == 0:
                return xh_d if pi == 0 else x_d[pi * NT - 128:pi * NT, :]
            if i <= TT:
                return x_d[pi * NT + (i - 1) * 128:pi * NT + i * 128, :]
            j = i - TT1
            return mem_d[j * 128:(j + 1) * 128, :]

        g1b = cp[:, G1C:G1C + 8].unsqueeze(2).to_broadcast([128, 8, 128])
        gmb = cp[:, GMC:GMC + 8].unsqueeze(2).to_broadcast([128, 8, 128])
        b_rs = [Buf() for _ in range(NTILES)]
        for i in range(NTILES):
            k = i % 3
            q2 = i % 2
            P.op("sp", lambda e, k=k, i=i: e.dma_start(out=xt[k], in_=src_tile(i)), writes=[b_xt[k]], dma=s_xt[k])
            P.op("act", lambda e, k=k, i=i: e.activation(out=junk, in_=xt[k], func=ACT.Square, accum_out=ss_all[:, i:i + 1]),
                 reads=[b_xt[k]], writes=[b_junk, b_rs[i]])
            P.op("act", lambda e, i=i: e.activation(out=lnt[:, i:i + 1], in_=ss_all[:, i:i + 1], func=ACT.Ln, scale=1.0 / D, bias=eps6[:, 0:1]),
                 reads=[b_const], writes=[b_rs[i]])
            P.op("act", lambda e, i=i: e.activation(out=rstd_all[:, i:i + 1], in_=lnt[:, i:i + 1], func=ACT.Exp, scale=-0.5), writes=[b_rs[i]])
            P.op("dve", lambda e, k=k, i=i, q2=q2: e.tensor_scalar(out=xn[q2], in0=xt[k], scalar1=rstd_all[:, i:i + 1], scalar2=None, op0=ALU.mult),
                 reads=[b_xt[k], b_rs[i]], writes=[b_xn[q2]])

            def tr(e, q2=q2):
                ins = None
                for c in range(8):
                    ins = e.transpose(out=psb(q2)[:, c * 128:(c + 1) * 128], in_=xn[q2][:, c * 128:(c + 1) * 128], identity=identb)
                return ins
            P.op("pe", tr, reads=[b_xn[q2], b_const], writes=[PB[q2]])
            src = psb(q2).rearrange("p (c t) -> p c t", c=8)
            if i <= TT:
                P.op("dve", lambda e, i=i, src=src: e.tensor_tensor(out=hT[:, :, i * 128:(i + 1) * 128], in0=src, in1=g1b, op=ALU.mult),
                     reads=[PB[q2], b_cp], writes=[b_hT[i]])
            else:
                j = i - TT1
                P.op("dve", lambda e, j=j, src=src: e.tensor_tensor(out=memT[:, :, j * 128:(j + 1) * 128], in0=src, in1=gmb, op=ALU.mult),
                     reads=[PB[q2], b_cp], writes=[b_memT])
        P.barrier()
        maybe_stop("p1a")
        AR.lo = mark1b

        sqm = AR.left([128, 512], F32)
        t1m = AR.left([128, 512], F32)
        mkn = AR.left([128, 512], BF16)
        ssk = AR.left([128, 4], F32)
        lk = AR.left([128, 4], F32)
        rk = AR.left([128, 4], F32)
        b_m = Buf("memscratch")
        for mt in range(2):
            def mm(e, mt=mt):
                ins = None
                for half in range(2):
                    for kc in range(8):
                        ins = e.matmul(out=psf(half), lhsT=memT[:, kc, mt * 128:(mt + 1) * 128], rhs=wkv[:, kc, half * 512:(half + 1) * 512],
                                       start=(kc == 0), stop=(kc == 7))
                return ins
            P.op("pe", mm, reads=[b_memT, b_wkv], writes=[PB[0], PB[1]])
            P.op("act", lambda e: e.activation(out=sqm, in_=psf(0), func=ACT.Square), reads=[PB[0]], writes=[b_m])
            P.op("dve", lambda e: e.tensor_reduce(out=ssk, in_=sqm.rearrange("p (h d) -> p h d", h=4), axis=AX.X, op=ALU.add), reads=[b_m], writes=[b_m])
            P.op("act", lambda e: e.activation(out=lk, in_=ssk, func=ACT.Ln, scale=1.0 / 128, bias=eps6[:, 0:1]), reads=[b_m, b_const], writes=[b_m])
            P.op("act", lambda e: e.activation(out=rk, in_=lk, func=ACT.Exp, scale=-0.5), reads=[b_m], writes=[b_m])
            P.op("dve", lambda e: e.tensor_tensor(out=t1m.rearrange("p (h d) -> p h d", h=4), in0=psf(0).rearrange("p (h d) -> p h d", h=4),
                                                  in1=rk.unsqueeze(2).to_broadcast([128, 4, 128]), op=ALU.mult), reads=[PB[0], b_m], writes=[b_m])
            P.op("dve", lambda e: e.tensor_tensor(out=mkn, in0=t1m, in1=cp[:, GXK:GXK + 512], op=ALU.mult), reads=[b_m, b_cp], writes=[b_m])

            def trk(e):
                ins = None
                for h in range(4):
                    ins = e.transpose(out=psb(2)[:, h * 128:(h + 1) * 128], in_=mkn[:, h * 128:(h + 1) * 128], identity=identb)
                return ins
            P.op("pe", trk, reads=[b_m, b_const], writes=[PB[2]])
            P.op("act", lambda e, mt=mt: e.activation(out=mkT[:, :, mt * 128:(mt + 1) * 128], in_=psb(2)[:, 0:512].rearrange("p (h t) -> p h t", h=4), func=ACT.Copy),
                 reads=[PB[2]], writes=[b_mkT])
            P.op("act", lambda e, mt=mt: e.activation(out=mv_sb[:, mt, :], in_=psf(1), func=ACT.Copy), reads=[PB[1]], writes=[b_mv])
        P.barrier()
        maybe_stop("p1b")
        AR.lo = mark1

        def cast_dma(dst, src, b, s):
            P.op("pool", lambda e: e.dma_start(out=dst, in_=src), writes=[b], dma=s)

        def gw_src(goff, s_):
            return w_in_d[:, goff + s_ * 256:goff + (s_ + 1) * 256].rearrange("(kc p) n -> p kc n", p=128)

        def merge_prefetch(w_o_d, goff):
            Wreset()
            wo_ = W([128, 4, 1024], BF16)
            gw_ = [W([128, 8, 256], BF16) for _ in range(2)]
            cast_dma(wo_, w_o_d.rearrange("(kc p) n -> p kc n", p=128), b_wo, s_wo)
            for s_ in range(2):
                cast_dma(gw_[s_], gw_src(goff, s_), b_gw[s_], s_gw[s_])
            return wo_, gw_

        def branch_merge(w_o_d, goff, first, pre=None):
            if pre is None:
                Wreset()
                wo = W([128, 4, 1024], BF16)
                gw = [W([128, 8, 256], BF16) for _ in range(2)]
                cast_dma(wo, w_o_d.rearrange("(kc p) n -> p kc n", p=128), b_wo, s_wo)
            else:
                wo, gw = pre
            tmp = [AR.left([128, 512], F32) for _ in range(2)]
            sg = [AR.left([128, 512], F32) for _ in range(2)]
            b_tmp = [Buf(), Buf()]
            b_sg = [Buf(), Buf()]
            it = 0
            for s in range(4):
                gb_ = s % 2
                if pre is None or s >= 2:
                    cast_dma(gw[gb_], gw_src(goff, s), b_gw[gb_], s_gw[gb_])
                for mi in range(2):
                    m = 2 * s + mi
                    for n in range(NS):
                        pp = it % 2
                        it += 1
                        yb, gbk = 4 + pp, 6 + pp

                        def mmy(e, m=m, n=n, yb=yb):
                            ins = None
                            for kc in range(4):
                                ins = e.matmul(out=psf(yb), lhsT=wo[:, kc, m * 128:(m + 1) * 128], rhs=bT[:, kc, n * 512:(n + 1) * 512],
                                               start=(kc == 0), stop=(kc == 3))
                            return ins
                        P.op("pe", mmy, reads=[b_wo] + b_bT[n * 4:(n + 1) * 4], writes=[PB[yb]])

                        def mmg(e, mi=mi, n=n, gbk=gbk, gb_=gb_):
                            ins = None
                            for kc in range(8):
                                ins = e.matmul(out=psf(gbk), lhsT=gw[gb_][:, kc, mi * 128:(mi + 1) * 128],
                                               rhs=hT[:, kc, 128 + n * 512:128 + (n + 1) * 512], start=(kc == 0), stop=(kc == 7))
                            return ins
                        P.op("pe", mmg, reads=[b_gw[gb_]] + b_hT[1 + n * 4:1 + (n + 1) * 4], writes=[PB[gbk]])
                        P.op("act", lambda e, gbk=gbk, pp=pp: e.activation(out=sg[pp], in_=psf(gbk), func=ACT.Sigmoid), reads=[PB[gbk]], writes=[b_sg[pp]])
                        dst = merged[:, m, n * 512:(n + 1) * 512]
                        if first:
                            P.op("dve", lambda e, yb=yb, pp=pp, dst=dst: e.tensor_tensor(out=dst, in0=psf(yb), in1=sg[pp], op=ALU.mult),
                                 reads=[PB[yb], b_sg[pp]], writes=[b_merged[m][n]])
                        else:
                            P.op("dve", lambda e, yb=yb, pp=pp: e.tensor_tensor(out=tmp[pp], in0=psf(yb), in1=sg[pp], op=ALU.mult),
                                 reads=[PB[yb], b_sg[pp]], writes=[b_tmp[pp]])
                            P.op("dve", lambda e, pp=pp, dst=dst: e.tensor_tensor(out=dst, in0=dst, in1=tmp[pp], op=ALU.add),
                                 reads=[b_tmp[pp]], writes=[b_merged[m][n]])

        pre_attn = merge_prefetch(woa_d, OFF_GATE)
        kT_all = AR.left([128, TT1 * 128], BF16)
        vaug = AR.left([128, TT1, 2, 65], BF16)
        b_kT = [Buf() for _ in range(TT1)]
        b_v = [Buf() for _ in range(TT1)]
        NB = 3
        sqs = [AR.left([128, 640], F32) for _ in range(NB)]
        ss10 = [AR.left([128, 10], F32) for _ in range(NB)]
        l10 = [AR.left([128, 10], F32) for _ in range(NB)]
        r10 = [AR.left([128, 10], F32) for _ in range(NB)]
        t1 = [AR.left([128, 10, 64], F32) for _ in range(NB)]
        t2 = [AR.left([128, 10, 64], F32) for _ in range(NB)]
        ra = [AR.left([128, 10, 32], F32) for _ in range(NB)]
        rb = [AR.left([128, 10, 32], F32) for _ in range(NB)]
        rc = [AR.left([128, 10, 32], F32) for _ in range(NB)]
        rd = [AR.left([128, 10, 32], F32) for _ in range(NB)]
        qkb = [AR.left([128, 640], BF16) for _ in range(NB)]
        qT = [AR.left([128, 512], BF16) for _ in range(2)]
        b_qT = [Buf(), Buf()]
        Pt = [AR.left([128, 512], BF16) for _ in range(2)]
        Pm = [AR.left([128, 512], BF16) for _ in range(4)]
        b_Pt = [Buf(), Buf()]
        b_Pm = [Buf() for _ in range(4)]
        den = [AR.left([128, 8], F32) for _ in range(NB)]
        rden = [AR.left([128, 8], F32) for _ in range(NB)]
        On = [AR.left([128, 8, 64], BF16) for _ in range(NB)]
        b_sq = [Buf() for _ in range(NB)]
        b_r10 = [Buf() for _ in range(NB)]
        b_t1 = [Buf() for _ in range(NB)]
        b_t2 = [Buf() for _ in range(NB)]
        b_ra = [Buf() for _ in range(NB)]
        b_rb = [Buf() for _ in range(NB)]
        b_rc = [Buf() for _ in range(NB)]
        b_rd = [Buf() for _ in range(NB)]
        b_qkb = [Buf() for _ in range(NB)]
        b_On = [Buf() for _ in range(NB)]
        b_den = [Buf() for _ in range(NB)]

        P.op("pool", lambda e: e.memset(vaug[:, :, :, 64:65], 1.0), writes=b_v)
        P.op("dve", lambda e: e.tensor_copy(out=vaug[:, 0, :, 64:65], in_=(cp[:, HFLAG:HFLAG + 1] if pi == 0 else ones_f[:, 0:1]).unsqueeze(1).to_broadcast([128, 2, 1])),
             reads=[b_cp], writes=[b_v[0]])
        QB, KVB, TB6, TB7 = 0, 1, 6, 7

        def stageA1(t):
            z = t % NB

            def mmq(e):
                ins = None
                for kc in range(8):
                    ins = e.matmul(out=psf(QB), lhsT=hT[:, kc, t * 128:(t + 1) * 128], rhs=wA[:, kc, 0:512], start=(kc == 0), stop=(kc == 7))
                for kc in range(8):
                    ins = e.matmul(out=psf(KVB)[:, 0:256], lhsT=hT[:, kc, t * 128:(t + 1) * 128], rhs=wA[:, kc, 512:768], start=(kc == 0), stop=(kc == 7))
                return ins
            P.op("pe", mmq, reads=[b_hT[t], b_wA], writes=[PB[QB], PB[KVB]])
            P.op("act", lambda e: e.activation(out=sqs[z][:, 0:512], in_=psf(QB), func=ACT.Square), reads=[PB[QB]], writes=[b_sq[z]])
            P.op("act", lambda e: e.activation(out=sqs[z][:, 512:640], in_=psf(KVB)[:, 0:128], func=ACT.Square), reads=[PB[KVB]], writes=[b_sq[z]])
            P.op("act", lambda e: e.activation(out=vaug[:, t, :, 0:64], in_=psf(KVB)[:, 128:256].rearrange("p (g d) -> p g d", g=2), func=ACT.Copy),
                 reads=[PB[KVB]], writes=[b_v[t]])
            P.op("dve", lambda e: e.tensor_reduce(out=ss10[z], in_=sqs[z].rearrange("p (h d) -> p h d", h=10), axis=AX.X, op=ALU.add), reads=[b_sq[z]], writes=[b_r10[z]])
            P.op("act", lambda e: e.activation(out=l10[z], in_=ss10[z], func=ACT.Ln, scale=1.0 / 64, bias=eps6[:, 0:1]), reads=[b_r10[z], b_const], writes=[b_r10[z]])
            P.op("act", lambda e: e.activation(out=r10[z], in_=l10[z], func=ACT.Exp, scale=-0.5), reads=[b_r10[z]], writes=[b_r10[z]])
            P.op("dve", lambda e: e.tensor_tensor(out=t1[z][:, 0:8, :], in0=psf(QB).rearrange("p (h d) -> p h d", h=8),
                                                  in1=r10[z][:, 0:8].unsqueeze(2).to_broadcast([128, 8, 64]), op=ALU.mult), reads=[PB[QB], b_r10[z]], writes=[b_t1[z]])
            P.op("dve", lambda e: e.tensor_tensor(out=t1[z][:, 8:10, :], in0=psf(KVB)[:, 0:128].rearrange("p (h d) -> p h d", h=2),
                                                  in1=r10[z][:, 8:10].unsqueeze(2).to_broadcast([128, 2, 64]), op=ALU.mult), reads=[PB[KVB], b_r10[z]], writes=[b_t1[z]])
            P.op("pool", lambda e: e.tensor_tensor(out=t2[z], in0=t1[z], in1=cp[:, GQK:GQK + 640].rearrange("p (h d) -> p h d", h=10), op=ALU.mult),
                 reads=[b_t1[z], b_cp], writes=[b_t2[z]])
            cb = cos_t[:, t, :].unsqueeze(1).to_broadcast([128, 10, 32])
            sb = sin_t[:, t, :].unsqueeze(1).to_broadcast([128, 10, 32])
            P.op("dve", lambda e: e.tensor_tensor(out=ra[z], in0=t2[z][:, :, 0:32], in1=cb, op=ALU.mult), reads=[b_t2[z], b_const], writes=[b_ra[z]])
            P.op("pool", lambda e: e.tensor_tensor(out=rb[z], in0=t2[z][:, :, 32:64], in1=sb, op=ALU.mult), reads=[b_t2[z], b_const], writes=[b_rb[z]])
            P.op("dve", lambda e: e.tensor_tensor(out=rc[z], in0=t2[z][:, :, 32:64], in1=cb, op=ALU.mult), reads=[b_t2[z], b_const], writes=[b_rc[z]])
            P.op("pool", lambda e: e.tensor_tensor(out=rd[z], in0=t2[z][:, :, 0:32], in1=sb, op=ALU.mult), reads=[b_t2[z], b_const], writes=[b_rd[z]])
            qdst = qkb[z][:, 0:512].rearrange("p (c g d) -> p g c d", c=4, g=2)
            kdst = qkb[z][:, 512:640].rearrange("p (h d) -> p h d", h=2)

            def v4(a):
                return a[:, 0:8, :].rearrange("p (g c) d -> p g c d", g=2)
            P.op("dve", lambda e: e.tensor_tensor(out=qdst[:, :, :, 0:32], in0=v4(ra[z]), in1=v4(rb[z]), op=ALU.subtract), reads=[b_ra[z], b_rb[z]], writes=[b_qkb[z]])
            P.op("dve", lambda e: e.tensor_tensor(out=qdst[:, :, :, 32:64], in0=v4(rc[z]), in1=v4(rd[z]), op=ALU.add), reads=[b_rc[z], b_rd[z]], writes=[b_qkb[z]])
            P.op("dve", lambda e: e.tensor_tensor(out=kdst[:, :, 0:32], in0=ra[z][:, 8:10, :], in1=rb[z][:, 8:10, :], op=ALU.subtract), reads=[b_ra[z], b_rb[z]], writes=[b_qkb[z]])
            P.op("dve", lambda e: e.tensor_tensor(out=kdst[:, :, 32:64], in0=rc[z][:, 8:10, :], in1=rd[z][:, 8:10, :], op=ALU.add), reads=[b_rc[z], b_rd[z]], writes=[b_qkb[z]])

        def stageA2(t):
            z = t % NB
            qq = t % 2

            def trq(e):
                ins = None
                for c in range(5):
                    ins = e.transpose(out=psb(TB6)[:, c * 128:(c + 1) * 128], in_=qkb[z][:, c * 128:(c + 1) * 128], identity=identb)
                return ins
            P.op("pe", trq, reads=[b_qkb[z], b_const], writes=[PB[TB6]])
            P.op("act", lambda e: e.activation(out=qT[qq], in_=psb(TB6)[:, 0:512], func=ACT.Copy), reads=[PB[TB6]], writes=[b_qT[qq]])
            P.op("dve", lambda e: e.tensor_copy(out=kT_all[:, t * 128:(t + 1) * 128], in_=psb(TB6)[:, 512:640]), reads=[PB[TB6]], writes=[b_kT[t]])

        def stageB(t):
            z = t % NB
            qq = t % 2

            def rec_S(g, jj):
                j = (t - 1, t)[jj]
                sbk = 2 + jj
                pt = jj
                pm = 2 * g + jj
                msk = (maskp4 if jj == 0 else maskc4)

                def mms(e):
                    e.matmul(out=psf(sbk), lhsT=kT_all[g * 64:(g + 1) * 64, j * 128:(j + 1) * 128],
                             rhs=qT[qq][g * 64:(g + 1) * 64, :], start=True, stop=False)
                    return e.matmul(out=psf(sbk), lhsT=identb, rhs=msk.rearrange("p a b -> p (a b)"), start=False, stop=True)
                P.op("pe", mms, reads=[b_kT[j], b_qT[qq], b_const], writes=[PB[sbk]])
                P.op("act", lambda e: e.activation(out=Pm[pm], in_=psf(sbk), func=ACT.Exp, scale=0.125), reads=[PB[sbk]], writes=[b_Pm[pm]])

            def rec_PV(g, jj):
                j = (t - 1, t)[jj]
                pm = 2 * g + jj
                ob = 4 + g

                def pv(e):
                    ins = None
                    for c in range(4):
                        ins = e.matmul(out=psf(ob)[:, c * 65:(c + 1) * 65], lhsT=Pm[pm][:, c * 128:(c + 1) * 128], rhs=vaug[:, j, g, :],
                                       start=(jj == 0 and c == 0), stop=(jj == 1 and c == 3), skip_group_check=True)
                    return ins
                P.op("pe", pv, reads=[b_Pm[pm], b_v[j]], writes=[PB[ob]])
            rec_S(0, 0)
            rec_S(0, 1)
            rec_PV(0, 0)
            rec_S(1, 0)
            rec_PV(0, 1)
            rec_S(1, 1)
            rec_PV(1, 0)
            rec_PV(1, 1)
            for g in range(2):
                ob = 4 + g
                ov = psf(ob)[:, 0:260].rearrange("p (c d) -> p c d", c=4)
                P.op("dve", lambda e, g=g, ov=ov: e.tensor_tensor(out=den[z][:, g * 4:(g + 1) * 4], in0=ov[:, :, 64], in1=esink[:, g * 4:(g + 1) * 4], op=ALU.add),
                     reads=[PB[ob], b_const], writes=[b_den[z]])
            P.op("dve", lambda e: e.reciprocal(out=rden[z], in_=den[z]), reads=[b_den[z]], writes=[b_den[z]])
            for g in range(2):
                ob = 4 + g
                ov = psf(ob)[:, 0:260].rearrange("p (c d) -> p c d", c=4)
                P.op("dve", lambda e, g=g, ov=ov: e.tensor_tensor(out=On[z][:, g * 4:(g + 1) * 4, :], in0=ov[:, :, 0:64],
                                                                  in1=rden[z][:, g * 4:(g + 1) * 4].unsqueeze(2).to_broadcast([128, 4, 64]), op=ALU.mult),
                     reads=[PB[ob], b_den[z]], writes=[b_On[z]])

            def tro(e):
                ins = None
                of = On[z].rearrange("p h d -> p (h d)")
                for c in range(4):
                    ins = e.transpose(out=psb(TB7)[:, c * 128:(c + 1) * 128], in_=of[:, c * 128:(c + 1) * 128], identity=identb)
                return ins
            P.op("pe", tro, reads=[b_On[z], b_const], writes=[PB[TB7]])
            P.op("act", lambda e: e.activation(out=bT[:, :, (t - 1) * 128:t * 128], in_=psb(TB7)[:, 0:512].rearrange("p (c t) -> p c t", c=4), func=ACT.Copy),
                 reads=[PB[TB7]], writes=[b_bT[t - 1]])

        stageA1(0)
        stageA1(1)
        stageA2(0)
        if TT1 > 2:
            stageA1(2)
        stageA2(1)
        for t in range(1, TT1):
            if t + 2 < TT1:
                stageA1(t + 2)
            stageB(t)
            if t + 1 < TT1:
                stageA2(t + 1)
        P.barrier()
        maybe_stop("attn")
        mark_att = AR.lo
        AR.lo = mark1
        branch_merge(woa_d, OFF_GATE, True, pre=pre_attn)
        P.barrier()
        maybe_stop("attn_merge")
        AR.lo = mark1

        Wreset()
        diag = W([128, 4, 31, 128], BF16)
        b_diag = [Buf() for _ in range(4)]
        w_after_diag = wstate["ptr"]
        uT = AR.left([128, 4, NTH], BF16)
        b_uT = [[Buf() for _ in range(NS + 1)] for _ in range(4)]
        mark_cv = AR.lo
        wB = [W([128, 8, 256], BF16) for _ in range(2)]
        b_wB = [Buf(), Buf()]
        s_wB = [P.dma_sem("wB0"), P.dma_sem("wB1")]
        sgb = [AR.left([128, 512], F32) for _ in range(2)]
        b_sgb = [Buf(), Buf()]

        def issue_wB(c):
            wb = c % 2
            P.op("pool", lambda e: e.dma_start(out=wB[wb][:, :, 0:128],
                                               in_=w_in_d[:, OFF_GLU + c * 128:OFF_GLU + (c + 1) * 128].rearrange("(kc p) n -> p kc n", p=128)),
                 writes=[b_wB[wb]], dma=s_wB[wb])
            P.op("pool", lambda e: e.dma_start(out=wB[wb][:, :, 128:256],
                                               in_=w_in_d[:, OFF_GLU + 512 + c * 128:OFF_GLU + 512 + (c + 1) * 128].rearrange("(kc p) n -> p kc n", p=128)),
                 writes=[b_wB[wb]], dma=s_wB[wb])
        issue_wB(0)
        issue_wB(1)
        for c in range(4):
            eng = "dve" if c % 2 == 0 else "pool"
            P.op(eng, lambda e, c=c: e.tensor_tensor(out=diag[:, c], in0=identb.unsqueeze(1).to_broadcast([128, 31, 128]),
                                                     in1=cp[:, WDW + c * 31:WDW + (c + 1) * 31].unsqueeze(2).to_broadcast([128, 31, 128]), op=ALU.mult),
                 reads=[b_const, b_cp], writes=[b_diag[c]])
        segs = [(0, 128)] + [(128 + n * 512, 512) for n in range(NS)]
        it = 0
        for c in range(4):
            wb = c % 2
            if c >= 2:
                issue_wB(c)
            for si, (st, ln) in enumerate(segs):
                pp = it % 2
                it += 1
                ga, gbk = 0 + pp, 2 + pp
                hb = [b_hT[0]] if si == 0 else b_hT[1 + (si - 1) * 4:1 + si * 4]

                def mmglu(e, wb=wb, st=st, ln=ln, ga=ga, gbk=gbk):
                    ins = None
                    for kc in range(8):
                        ins = e.matmul(out=psf(ga)[:, 0:ln], lhsT=wB[wb][:, kc, 0:128], rhs=hT[:, kc, st:st + ln], start=(kc == 0), stop=(kc == 7))
                    for kc in range(8):
                        ins = e.matmul(out=psf(gbk)[:, 0:ln], lhsT=wB[wb][:, kc, 128:256], rhs=hT[:, kc, st:st + ln], start=(kc == 0), stop=(kc == 7))
                    return ins
                P.op("pe", mmglu, reads=[b_wB[wb]] + hb, writes=[PB[ga], PB[gbk]])
                P.op("act", lambda e, gbk=gbk, pp=pp, ln=ln: e.activation(out=sgb[pp][:, 0:ln], in_=psf(gbk)[:, 0:ln], func=ACT.Sigmoid), reads=[PB[gbk]], writes=[b_sgb[pp]])
                P.op("dve", lambda e, ga=ga, pp=pp, c=c, st=st, ln=ln: e.tensor_tensor(out=uT[:, c, st:st + ln], in0=psf(ga)[:, 0:ln], in1=sgb[pp][:, 0:ln], op=ALU.mult),
                     reads=[PB[ga], b_sgb[pp]], writes=[b_uT[c][si]])
        P.barrier()
        AR.lo = mark_cv
        wstate["ptr"] = w_after_diag
        vv2 = [W([128, 4, 512], F32)]
        sqv2 = [W([128, 4, 512], F32)]
        var = W([128, 512], F32)
        rsd = W([128, 512], F32)
        mean = AR.left([128, 512], F32)
        dd = [AR.left([128, 512], F32) for _ in range(3)]
        vv2.append(AR.left([128, 4, 512], F32))
        sqv2.append(AR.left([128, 4, 512], F32))
        b_vv2 = [[Buf() for _ in range(4)] for _ in range(2)]
        b_sqv2 = [[Buf() for _ in range(4)] for _ in range(2)]
        b_dd = [Buf(), Buf(), Buf()]
        m2 = dd[2]
        lv = var
        b_st = Buf("lnstats")

        def stageC(n):
            z = n % 2
            vv, sqv, b_vv, b_sqv = vv2[z], sqv2[z], b_vv2[z], b_sqv2[z]
            for c in range(4):
                cvb = 4 + (c % 2)

                def mmconv(e, c=c, cvb=cvb):
                    ins = None
                    for j in range(31):
                        s0 = 128 + n * 512 - 30 + j
                        ins = e.matmul(out=psf(cvb), lhsT=diag[:, c, j, :], rhs=uT[:, c, s0:s0 + 512], start=(j == 0), stop=(j == 30))
                    return ins
                P.op("pe", mmconv, reads=[b_diag[c], b_uT[c][n], b_uT[c][n + 1]], writes=[PB[cvb]])
                P.op("act", lambda e, c=c, cvb=cvb: e.activation(out=vv[:, c, :], in_=psf(cvb), func=ACT.Identity, bias=cp[:, BDW + c:BDW + c + 1], scale=1.0),
                     reads=[PB[cvb], b_cp], writes=[b_vv[c]])
                P.op("pool", lambda e, c=c: e.tensor_tensor(out=sqv[:, c, :], in0=vv[:, c, :], in1=vv[:, c, :], op=ALU.mult), reads=[b_vv[c]], writes=[b_sqv[c]])

            sa, sq_ = (6, 7) if z == 0 else (2, 3)

            def mmst(e):
                ins = None
                for c in range(4):
                    ins = e.matmul(out=psf(sa), lhsT=ones_f, rhs=vv[:, c, :], start=(c == 0), stop=(c == 3))
                for c in range(4):
                    ins = e.matmul(out=psf(sq_), lhsT=ones_f, rhs=sqv[:, c, :], start=(c == 0), stop=(c == 3))
                return ins
            P.op("pe", mmst, reads=b_vv + b_sqv + [b_const], writes=[PB[sa], PB[sq_]])

        def stageL(n):
            z = n % 2
            vv, b_vv = vv2[z], b_vv2[z]
            sa, sq_ = (6, 7) if z == 0 else (2, 3)
            P.op("dve", lambda e: e.tensor_scalar(out=mean, in0=psf(sa), scalar1=1.0 / 512, scalar2=None, op0=ALU.mult), reads=[PB[sa]], writes=[b_st])
            P.op("dve", lambda e: e.tensor_tensor(out=m2, in0=mean, in1=mean, op=ALU.mult), reads=[b_st], writes=[b_st])
            P.op("dve", lambda e: e.scalar_tensor_tensor(out=var, in0=psf(sq_), scalar=1.0 / 512, in1=m2, op0=ALU.mult, op1=ALU.subtract), reads=[PB[sq_], b_st], writes=[b_st])
            P.op("act", lambda e: e.activation(out=lv, in_=var, func=ACT.Ln, bias=eps5[:, 0:1], scale=1.0), reads=[b_st, b_const], writes=[b_st])
            P.op("act", lambda e: e.activation(out=rsd, in_=lv, func=ACT.Exp, scale=-0.5), reads=[b_st], writes=[b_st])
            for c in range(4):
                pp = c % 2
                P.op("dve", lambda e, c=c, pp=pp: e.tensor_tensor(out=dd[pp], in0=vv[:, c, :], in1=mean, op=ALU.subtract), reads=[b_vv[c], b_st], writes=[b_dd[pp]])
                P.op("dve", lambda e, pp=pp: e.tensor_tensor(out=dd[pp], in0=dd[pp], in1=rsd, op=ALU.mult), reads=[b_st], writes=[b_dd[pp]])
                P.op("act", lambda e, c=c, pp=pp: e.activation(out=bT[:, c, n * 512:(n + 1) * 512], in_=dd[pp], func=ACT.Silu,
                                                               scale=cp[:, GLN + c:GLN + c + 1], bias=cp[:, BLN + c:BLN + c + 1]),
                     reads=[b_dd[pp], b_cp], writes=b_bT[n * 4:(n + 1) * 4])

        stageC(0)
        for n in range(NS):
            if n + 1 < NS:
                stageC(n + 1)
            stageL(n)
        P.barrier()
        maybe_stop("conv")
        AR.lo = mark1
        branch_merge(wco_d, OFF_GATE + 1024, False)
        P.barrier()
        maybe_stop("conv_merge")
        AR.lo = mark1

        pre_mem = merge_prefetch(wom_d, OFF_GATE + 2048)
        wC = W([128, 8, 512], BF16)
        b_wC = Buf("wC")
        s_wC = P.dma_sem("wC")
        xqT = W([128, 4, NT], BF16)
        b_xqT = [Buf() for _ in range(TT)]
        sqx = [W([128, 512], F32) for _ in range(2)]
        t1x = [W([128, 512], F32) for _ in range(2)]
        xqn = [W([128, 512], BF16) for _ in range(2)]
        ss4 = [W([128, 4], F32) for _ in range(2)]
        l4 = [W([128, 4], F32) for _ in range(2)]
        r4 = [W([128, 4], F32) for _ in range(2)]
        Pmx = [W([128, 512], BF16) for _ in range(4)]
        b_Pmx = [Buf() for _ in range(4)]
        rdx = [W([128, 512], F32) for _ in range(2)]
        b_rdx = [Buf(), Buf()]
        b_x = [Buf(), Buf()]
        b_xqn = [Buf(), Buf()]
        cast_dma(wC, w_in_d[:, OFF_XQ:OFF_XQ + 512].rearrange("(kc p) n -> p kc n", p=128), b_wC, s_wC)

        def stageQ1(t):
            z = t % 2
            xb = t % 2

            def mmxq(e):
                ins = None
                for kc in range(8):
                    ins = e.matmul(out=psf(xb), lhsT=hT[:, kc, (t + 1) * 128:(t + 2) * 128], rhs=wC[:, kc, :], start=(kc == 0), stop=(kc == 7))
                return ins
            P.op("pe", mmxq, reads=[b_hT[t + 1], b_wC], writes=[PB[xb]])
            P.op("act", lambda e: e.activation(out=sqx[z], in_=psf(xb), func=ACT.Square), reads=[PB[xb]], writes=[b_x[z]])
            P.op("dve", lambda e: e.tensor_reduce(out=ss4[z], in_=sqx[z].rearrange("p (h d) -> p h d", h=4), axis=AX.X, op=ALU.add), reads=[b_x[z]], writes=[b_x[z]])
            P.op("act", lambda e: e.activation(out=l4[z], in_=ss4[z], func=ACT.Ln, scale=1.0 / 128, bias=eps6[:, 0:1]), reads=[b_x[z], b_const], writes=[b_x[z]])
            P.op("act", lambda e: e.activation(out=r4[z], in_=l4[z], func=ACT.Exp, scale=-0.5), reads=[b_x[z]], writes=[b_x[z]])
            P.op("dve", lambda e: e.tensor_tensor(out=t1x[z].rearrange("p (h d) -> p h d", h=4), in0=psf(xb).rearrange("p (h d) -> p h d", h=4),
                                                  in1=r4[z].unsqueeze(2).to_broadcast([128, 4, 128]), op=ALU.mult), reads=[PB[xb], b_x[z]], writes=[b_x[z]])
            P.op("pool", lambda e: e.tensor_tensor(out=xqn[z], in0=t1x[z], in1=cp[:, GXQ:GXQ + 512], op=ALU.mult), reads=[b_x[z], b_cp], writes=[b_xqn[z]])

        def stageQ2(t):
            z = t % 2

            def trx(e):
                ins = None
                for h in range(4):
                    ins = e.transpose(out=psb(2)[:, h * 128:(h + 1) * 128], in_=xqn[z][:, h * 128:(h + 1) * 128], identity=identb)
                return ins
            P.op("pe", trx, reads=[b_xqn[z], b_const], writes=[PB[2]])
            P.op("act", lambda e: e.activation(out=xqT[:, :, t * 128:(t + 1) * 128], in_=psb(2)[:, 0:512].rearrange("p (h t) -> p h t", h=4), func=ACT.Copy),
                 reads=[PB[2]], writes=[b_xqT[t]])

        stageQ1(0)
        for t in range(TT):
            if t + 1 < TT:
                stageQ1(t + 1)
            stageQ2(t)
        it = 0
        for n in range(NS):
            for h in range(4):
                pp = it % 2
                it += 1
                for mt in range(2):
                    sbk = 3 + mt
                    pm = 2 * pp + mt
                    P.op("pe", lambda e, h=h, n=n, mt=mt, sbk=sbk: e.matmul(out=psf(sbk), lhsT=mkT[:, h, mt * 128:(mt + 1) * 128],
                                                                            rhs=xqT[:, h, n * 512:(n + 1) * 512], start=True, stop=True),
                         reads=[b_mkT] + b_xqT[n * 4:(n + 1) * 4], writes=[PB[sbk]])
                    P.op("act", lambda e, sbk=sbk, pm=pm: e.activation(out=Pmx[pm], in_=psf(sbk), func=ACT.Exp, scale=128.0 ** -0.5), reads=[PB[sbk]], writes=[b_Pmx[pm]])
                ob, db = 5, 6 + pp

                def mmo(e, h=h, pp=pp, ob=ob, db=db):
                    ins = None
                    for mt in range(2):
                        ins = e.matmul(out=psf(ob), lhsT=mv_sb[:, mt, h * 128:(h + 1) * 128], rhs=Pmx[2 * pp + mt], start=(mt == 0), stop=(mt == 1))
                    for mt in range(2):
                        ins = e.matmul(out=psf(db), lhsT=ones_b, rhs=Pmx[2 * pp + mt], start=(mt == 0), stop=(mt == 1))
                    return ins
                P.op("pe", mmo, reads=[b_mv, b_const, b_Pmx[2 * pp], b_Pmx[2 * pp + 1]], writes=[PB[ob], PB[db]])
                P.op("dve", lambda e, pp=pp, db=db: e.reciprocal(out=rdx[pp], in_=psf(db)), reads=[PB[db]], writes=[b_rdx[pp]])
                P.op("dve", lambda e, pp=pp, ob=ob, h=h, n=n: e.tensor_tensor(out=bT[:, h, n * 512:(n + 1) * 512], in0=psf(ob), in1=rdx[pp], op=ALU.mult),
                     reads=[PB[ob], b_rdx[pp]], writes=b_bT[n * 4:(n + 1) * 4])
        P.barrier()
        maybe_stop("mem")
        AR.lo = mark1
        branch_merge(wom_d, OFF_GATE + 2048, False, pre=pre_mem)
        P.barrier()
        maybe_stop("mem_merge")
        if debug:
            s_dbg = P.dma_sem("dbg")
            b_dbg = Buf("dbg")
            P.op("sp", lambda e: e.dma_start(out=dbg["merged"], in_=merged.rearrange("p a b -> p (a b)")), writes=[b_dbg], dma=s_dbg)
            P.op("sp", lambda e: e.dma_start(out=dbg["hT"], in_=hT.rearrange("p a b -> p (a b)")), writes=[b_dbg], dma=s_dbg)
            P.barrier()

        AR.lo = mark0 + ((8 * NT * 2 + 63) // 64 * 64)
        x1acc = AR.right([128, TT, D], F32)
        h2T = AR.right([128, 8, NT], BF16)
        b_x1 = [[Buf(), Buf()] for _ in range(TT)]
        b_h2T = [Buf() for _ in range(TT)]
        Wreset()
        wout = W([128, 8, D], BF16)
        b_wout = Buf("wout")
        s_wout = P.dma_sem("wout")
        wg = [None, None]
        wu = [None, None]
        wd = [None, None]
        wg[0] = W([128, 8, FF], BF16)
        wu[0] = W([128, 8, FF], BF16)
        wd[0] = W([128, 4, D], BF16)
        wd[1] = W([128, 4, D], BF16)
        _save_ptr = wstate["ptr"]
        wstate["ptr"] = wstate["base"]
        wg[1] = W([128, 8, FF], BF16)
        wu[1] = W([128, 8, FF], BF16)
        wstate["ptr"] = _save_ptr
        b_wg = [Buf(), Buf()]
        b_wu = [Buf(), Buf()]
        b_wd = [Buf(), Buf()]
        s_wg = [P.dma_sem("wg0"), P.dma_sem("wg1")]
        s_wu = [P.dma_sem("wu0"), P.dma_sem("wu1")]
        s_wd = [P.dma_sem("wd0"), P.dma_sem("wd1")]

        def issue_expert(ex):
            b = ex % 2
            extra = [b_wout] if b == 1 else []
            P.op("pool", lambda e: e.dma_start(out=wg[b], in_=wg_d[ex].rearrange("(kc p) n -> p kc n", p=128)), writes=[b_wg[b]] + extra, dma=s_wg[b])
            P.op("pool", lambda e: e.dma_start(out=wu[b], in_=wu_d[ex].rearrange("(kc p) n -> p kc n", p=128)), writes=[b_wu[b]] + extra, dma=s_wu[b])
            P.op("pool", lambda e: e.dma_start(out=wd[b], in_=wd_d[ex].rearrange("(kc p) n -> p kc n", p=128)), writes=[b_wd[b]], dma=s_wd[b])
        wgr = AR.left([128, 8, 20], F32)
        b_wgr = Buf("wgr")
        s_wgr = P.dma_sem("wgr")
        xt2 = [AR.left([128, D], F32) for _ in range(2)]
        b_xt2 = [Buf(), Buf()]
        s_xt2 = [P.dma_sem("xt2a"), P.dma_sem("xt2b")]
        junk2 = AR.left([128, D], BF16)
        b_junk2 = Buf()
        ss2 = AR.left([128, TT], F32)
        l2 = AR.left([128, TT], F32)
        r2 = AR.left([128, TT], F32)
        b_r2 = [Buf() for _ in range(TT)]
        h2 = [AR.left([128, D], F32) for _ in range(2)]
        b_h2 = [Buf(), Buf()]
        h2Tf = [AR.left([128, 8, 128], F32) for _ in range(2)]
        b_h2Tf = [Buf(), Buf()]
        lg_all = AR.left([128, TT, 20], F32)
        b_lg = Buf("lg")
        cast_dma(wout, wout_d.rearrange("(kc p) n -> p kc n", p=128), b_wout, s_wout)
        if n_experts > 0:
            issue_expert(0)
        P.op("sp", lambda e: e.dma_start(out=wgr, in_=wgr_d.rearrange("(kc p) n -> p kc n", p=128)), writes=[b_wgr], dma=s_wgr)
        g2b = cp[:, G2C:G2C + 8].unsqueeze(2).to_broadcast([128, 8, 128])
        brt_b = cp[:, BRT:BRT + 20]
        def stageX(t):
            k = t % 2
            P.op("sp", lambda e, k=k, t=t: e.dma_start(out=xt2[k], in_=x_d[pi * NT + t * 128:pi * NT + (t + 1) * 128, :]), writes=[b_xt2[k]], dma=s_xt2[k])
            for half in range(2):
                xb = 0 + half

                def mmx1(e, t=t, half=half, xb=xb):
                    ins = None
                    for m in range(8):
                        ins = e.matmul(out=psf(xb), lhsT=merged[:, m, t * 128:(t + 1) * 128], rhs=wout[:, m, half * 512:(half + 1) * 512],
                                       start=(m == 0), stop=(m == 7))
                    return ins
                P.op("pe", mmx1, reads=[b_wout] + [b_merged[m][t // 4] for m in range(8)], writes=[PB[xb]])
                P.op("dve", lambda e, t=t, half=half, xb=xb, k=k: e.tensor_tensor(out=x1acc[:, t, half * 512:(half + 1) * 512], in0=psf(xb),
                                                                                 in1=xt2[k][:, half * 512:(half + 1) * 512], op=ALU.add),
                     reads=[PB[xb], b_xt2[k]], writes=[b_x1[t][half]])
            P.op("act", lambda e, t=t: e.activation(out=junk2, in_=x1acc[:, t, :], func=ACT.Square, accum_out=ss2[:, t:t + 1]),
                 reads=b_x1[t], writes=[b_junk2, b_r2[t]])
            P.op("act", lambda e, t=t: e.activation(out=l2[:, t:t + 1], in_=ss2[:, t:t + 1], func=ACT.Ln, scale=1.0 / D, bias=eps6[:, 0:1]), reads=[b_const], writes=[b_r2[t]])
            P.op("act", lambda e, t=t: e.activation(out=r2[:, t:t + 1], in_=l2[:, t:t + 1], func=ACT.Exp, scale=-0.5), writes=[b_r2[t]])
            P.op("dve", lambda e, t=t, k=k: e.tensor_scalar(out=h2[k], in0=x1acc[:, t, :], scalar1=r2[:, t:t + 1], scalar2=None, op0=ALU.mult),
                 reads=b_x1[t] + [b_r2[t]], writes=[b_h2[k]])


        def stageT(t):
            k = t % 2
            def trh(e, k=k):
                ins = None
                for c in range(8):
                    ins = e.transpose(out=psf(2 + c // 4)[:, (c % 4) * 128:(c % 4 + 1) * 128], in_=h2[k][:, c * 128:(c + 1) * 128], identity=identf)
                return ins
            P.op("pe", trh, reads=[b_h2[k], b_cp], writes=[PB[2], PB[3]])
            for hh in range(2):
                P.op("dve", lambda e, k=k, hh=hh: e.tensor_tensor(out=h2Tf[k][:, hh * 4:(hh + 1) * 4, :], in0=psf(2 + hh).rearrange("p (c t) -> p c t", c=4),
                                                                  in1=g2b[:, hh * 4:(hh + 1) * 4, :], op=ALU.mult),
                     reads=[PB[2 + hh], b_cp], writes=[b_h2Tf[k]])
            P.op("pool", lambda e, k=k, t=t: e.tensor_copy(out=h2T[:, :, t * 128:(t + 1) * 128], in_=h2Tf[k]), reads=[b_h2Tf[k]], writes=[b_h2T[t]])

            def mmlg(e, k=k):
                ins = None
                for kc in range(8):
                    ins = e.matmul(out=psf(4)[:, 0:20], lhsT=h2Tf[k][:, kc, :], rhs=wgr[:, kc, :], start=(kc == 0), stop=(kc == 7))
                return ins
            P.op("pe", mmlg, reads=[b_h2Tf[k], b_wgr], writes=[PB[4]])
            P.op("dve", lambda e, t=t: e.tensor_tensor(out=lg_all[:, t, :], in0=psf(4)[:, 0:20], in1=brt_b, op=ALU.add), reads=[PB[4], b_cp], writes=[b_lg])

        stageX(0)
        for t in range(TT):
            if t + 1 < TT:
                stageX(t + 1)
            stageT(t)

        def sm(shape):
            return AR.left(shape, F32)
        gl = lg_all[:, :, 0:4]
        el = lg_all[:, :, 4:20].rearrange("p t (g e) -> p t g e", g=4)
        gmax = sm([128, TT])
        ohg = sm([128, TT, 4])
        gd = sm([128, TT, 4])
        gex = sm([128, TT, 4])
        gsum = sm([128, TT])
        pg = sm([128, TT])
        elm = sm([128, TT, 4, 4])
        els = sm([128, TT, 4])
        m1 = sm([128, TT])
        oh1 = sm([128, TT, 4])
        els2 = sm([128, TT, 4])
        m2r = sm([128, TT])
        oh2 = sm([128, TT, 4])
        ddm = sm([128, TT])
        ee = sm([128, TT])
        s1 = sm([128, TT])
        p1 = sm([128, TT])
        p2 = sm([128, TT])
        wa = sm([128, TT, 4])
        wb2 = sm([128, TT, 4])
        wsel = sm([128, TT, 4])
        b_rt = Buf("route")

        def R(eng, fn, extra_r=(), w=None):
            P.op(eng, fn, reads=[b_rt, b_lg] + list(extra_r), writes=[b_rt] if w is None else w)

        def bc3(a):
            return a.unsqueeze(2).to_broadcast([128, TT, 4])
        R("dve", lambda e: e.tensor_reduce(out=gmax, in_=gl, axis=AX.X, op=ALU.max))
        R("dve", lambda e: e.tensor_tensor(out=ohg, in0=gl, in1=bc3(gmax), op=ALU.is_equal))
        R("dve", lambda e: e.tensor_tensor(out=gd, in0=gl, in1=bc3(gmax), op=ALU.subtract))
        R("act", lambda e: e.activation(out=gex, in_=gd, func=ACT.Exp))
        R("dve", lambda e: e.tensor_reduce(out=gsum, in_=gex, axis=AX.X, op=ALU.add))
        R("dve", lambda e: e.reciprocal(out=pg, in_=gsum))
        R("dve", lambda e: e.tensor_tensor(out=elm, in0=el, in1=ohg.unsqueeze(3).to_broadcast([128, TT, 4, 4]), op=ALU.mult))
        R("dve", lambda e: e.tensor_reduce(out=els, in_=elm.rearrange("p t g e -> p t e g"), axis=AX.X, op=ALU.add))
        R("dve", lambda e: e.tensor_reduce(out=m1, in_=els, axis=AX.X, op=ALU.max))
        R("dve", lambda e: e.tensor_tensor(out=oh1, in0=els, in1=bc3(m1), op=ALU.is_equal))
        R("dve", lambda e: e.scalar_tensor_tensor(out=els2, in0=oh1, scalar=-1e30, in1=els, op0=ALU.mult, op1=ALU.add))
        R("dve", lambda e: e.tensor_reduce(out=m2r, in_=els2, axis=AX.X, op=ALU.max))
        R("dve", lambda e: e.tensor_tensor(out=oh2, in0=els2, in1=bc3(m2r), op=ALU.is_equal))
        R("dve", lambda e: e.tensor_tensor(out=ddm, in0=m2r, in1=m1, op=ALU.subtract))
        R("act", lambda e: e.activation(out=ee, in_=ddm, func=ACT.Exp))
        R("dve", lambda e: e.tensor_scalar(out=s1, in0=ee, scalar1=1.0, scalar2=None, op0=ALU.add))
        R("dve", lambda e: e.reciprocal(out=p1, in_=s1))
        R("dve", lambda e: e.tensor_tensor(out=p2, in0=ee, in1=p1, op=ALU.mult))
        R("dve", lambda e: e.tensor_tensor(out=p1, in0=p1, in1=pg, op=ALU.mult))
        R("dve", lambda e: e.tensor_tensor(out=p2, in0=p2, in1=pg, op=ALU.mult))
        R("dve", lambda e: e.tensor_tensor(out=wa, in0=oh1, in1=bc3(p1), op=ALU.mult))
        R("dve", lambda e: e.tensor_tensor(out=wb2, in0=oh2, in1=bc3(p2), op=ALU.mult))
        R("dve", lambda e: e.tensor_tensor(out=wsel, in0=wa, in1=wb2, op=ALU.add))
        R("dve", lambda e: e.tensor_tensor(out=c_all.rearrange("p t (g e) -> p t g e", g=4), in0=ohg.unsqueeze(3).to_broadcast([128, TT, 4, 4]),
                                           in1=wsel.unsqueeze(2).to_broadcast([128, TT, 4, 4]), op=ALU.mult), w=[b_rt, b_call])
        if debug:
            P.barrier()
        if debug:
            for t in range(TT):
                P.op("sp", lambda e, t=t: e.dma_start(out=dbg["x1"][t * 128:(t + 1) * 128, :], in_=x1acc[:, t, :]), writes=[b_dbg], dma=s_dbg)
            P.op("sp", lambda e: e.dma_start(out=dbg["call"], in_=c_all.rearrange("p t e -> p (t e)")), writes=[b_dbg], dma=s_dbg)
            P.barrier()

        AR.lo = mark0
        hid = AR.left([128, 4, NT], BF16)
        b_hid = [[Buf() for _ in range(NS)] for _ in range(4)]
        sgm = [AR.left([128, 512], F32) for _ in range(2)]
        b_sgm = [Buf(), Buf()]
        it = 0
        iy = 0
        for ex in range(n_experts):
            b = ex % 2
            if ex > 0:
                issue_expert(ex)
            for n in range(NS):
                for f in range(4):
                    pp = it % 2
                    it += 1
                    gp, up = 0 + pp, 2 + pp

                    def mmgu(e, b=b, n=n, f=f, gp=gp, up=up):
                        ins = None
                        for kc in range(8):
                            ins = e.matmul(out=psf(gp), lhsT=wg[b][:, kc, f * 128:(f + 1) * 128], rhs=h2T[:, kc, n * 512:(n + 1) * 512], start=(kc == 0), stop=(kc == 7))
                        for kc in range(8):
                            ins = e.matmul(out=psf(up), lhsT=wu[b][:, kc, f * 128:(f + 1) * 128], rhs=h2T[:, kc, n * 512:(n + 1) * 512], start=(kc == 0), stop=(kc == 7))
                        return ins
                    P.op("pe", mmgu, reads=[b_wg[b], b_wu[b]] + b_h2T[n * 4:(n + 1) * 4], writes=[PB[gp], PB[up]])
                    P.op("act", lambda e, gp=gp, pp=pp: e.activation(out=sgm[pp], in_=psf(gp), func=ACT.Silu), reads=[PB[gp]], writes=[b_sgm[pp]])
                    P.op("dve", lambda e, up=up, pp=pp, f=f, n=n: e.tensor_tensor(out=hid[:, f, n * 512:(n + 1) * 512], in0=psf(up), in1=sgm[pp], op=ALU.mult),
                         reads=[PB[up], b_sgm[pp]], writes=[b_hid[f][n]])
            for t in range(TT):
                for half in range(2):
                    yb = 4 + iy % 4
                    iy += 1

                    def mmd(e, b=b, t=t, half=half, yb=yb):
                        ins = None
                        for f in range(4):
                            ins = e.matmul(out=psf(yb), lhsT=hid[:, f, t * 128:(t + 1) * 128], rhs=wd[b][:, f, half * 512:(half + 1) * 512], start=(f == 0), stop=(f == 3))
                        return ins
                    P.op("pe", mmd, reads=[b_wd[b]] + [b_hid[f][t // 4] for f in range(4)], writes=[PB[yb]])
                    dst = x1acc[:, t, half * 512:(half + 1) * 512]
                    P.op("dve", lambda e, yb=yb, t=t, ex=ex, dst=dst: e.scalar_tensor_tensor(out=dst, in0=psf(yb), scalar=c_all[:, t, ex:ex + 1], in1=dst,
                                                                                             op0=ALU.mult, op1=ALU.add),
                         reads=[PB[yb], b_call], writes=[b_x1[t][half]])
        s_out = [P.dma_sem("out%d" % i) for i in range(2)]
        b_out = [Buf(), Buf()]
        for t in range(TT):
            P.op("sp", lambda e, t=t: e.dma_start(out=out_d[pi * NT + t * 128:pi * NT + (t + 1) * 128, :], in_=x1acc[:, t, :]), reads=b_x1[t], writes=[b_out[t % 2]], dma=s_out[t % 2])
        P.barrier()

    for pi in (range(NPASS) if passes is None else passes):
        AR.lo = mark0
        AR.hi = arena_hi0
        run_pass(pi)
    P.barrier()
    P.emit()
    return nc


def make_cp(inp, first):
    f32 = np.float32
    cp = np.zeros((128, NCP), f32)
    cp[:, G1C:G1C + 8] = inp["g_norm1"][0].reshape(8, 128).T
    cp[:, G2C:G2C + 8] = inp["g_norm2"][0].reshape(8, 128).T
    cp[:, GMC:GMC + 8] = inp["g_mem"][0].reshape(8, 128).T
    cp[:, BDW:BDW + 4] = inp["b_conv_dw"][0].reshape(4, 128).T
    cp[:, GLN:GLN + 4] = inp["g_conv_ln"][0].reshape(4, 128).T
    cp[:, BLN:BLN + 4] = inp["b_conv_ln"][0].reshape(4, 128).T
    w = np.asarray(inp["w_conv_dw"][0])
    cp[:, WDW:WDW + 124] = w.T.reshape(4, 128, 31).transpose(1, 0, 2).reshape(128, 124)
    cp[:, GQK:GQK + 512] = np.tile(inp["g_q"][0], 8)[None, :]
    cp[:, GQK + 512:GQK + 640] = np.tile(inp["g_k"][0], 2)[None, :]
    cp[:, GXQ:GXQ + 512] = np.tile(inp["g_xq"][0], 4)[None, :]
    cp[:, GXK:GXK + 512] = np.tile(inp["g_xk"][0], 4)[None, :]
    cp[:, SINK:SINK + 8] = inp["sinks"][0][None, :]
    cp[:, BRT:BRT + 4] = inp["b_group"][0][None, :]
    cp[:, BRT + 4:BRT + 20] = inp["b_router"][0][None, :]
    inv_freq = (1.0 / (np.float32(10000.0) ** (np.arange(0, 64, 2, dtype=f32) / np.float32(64)))).astype(f32)
    cp[:, INVF:INVF + 32] = inv_freq[None, :]
    cp[:, IDENT:IDENT + 128] = np.eye(128, dtype=f32)
    kk = np.arange(128)[:, None]
    qq = np.arange(128)[None, :]
    cp[:, MASKC:MASKC + 128] = (kk <= qq).astype(f32)
    cp[:, HFLAG] = 0.0 if first else 1.0
    return cp


def make_in_maps(inp, NT, ncores):
    f32 = np.float32
    x = np.asarray(inp["x"], f32)
    B, S, _ = x.shape
    per_b = S // NT
    assert B * per_b == ncores
    pos = np.asarray(inp["positions"], np.int32)
    mem = np.asarray(inp["mem"], f32)
    shared = {
        "w_in": np.ascontiguousarray(inp["w_in"][0], f32),
        "w_o_attn": np.ascontiguousarray(inp["w_o_attn"][0], f32),
        "w_conv_out": np.ascontiguousarray(inp["w_conv_out"][0], f32),
        "w_kv_mem": np.ascontiguousarray(inp["w_kv_mem"][0], f32),
        "w_o_mem": np.ascontiguousarray(inp["w_o_mem"][0], f32),
        "w_out": np.ascontiguousarray(inp["w_out"][0], f32),
        "w_gr": np.ascontiguousarray(np.concatenate([inp["w_group"][0], inp["w_router"][0]], axis=1), f32),
        "w_gate": np.ascontiguousarray(inp["w_gate"][0], f32),
        "w_up": np.ascontiguousarray(inp["w_up"][0], f32),
        "w_down": np.ascontiguousarray(inp["w_down"][0], f32),
    }
    cps = {True: make_cp(inp, True), False: make_cp(inp, False)}
    maps = []
    for c in range(ncores):
        b, j = divmod(c, per_b)
        s0 = j * NT
        first = (j == 0)
        xh = np.zeros((128, D), f32) if first else x[b, s0 - 128:s0]
        ph = np.zeros((128,), np.int32) if first else pos[b, s0 - 128:s0]
        pall = np.concatenate([ph, pos[b, s0:s0 + NT]]).reshape(NT // 128 + 1, 128).T
        m = {"x": np.ascontiguousarray(x[b, s0:s0 + NT]), "xh": np.ascontiguousarray(xh), "pos": np.ascontiguousarray(pall, np.int32),
             "mem": np.ascontiguousarray(mem[b]), "cp": cps[first]}
        m.update(shared)
        maps.append(m)
    return maps


_NC_CACHE = {}


def kernel(**inputs):
    inp = {k: np.asarray(v) for k, v in inputs.items()}
    B, S, _ = inp["x"].shape
    NT = B * S // NCORES
    if NT not in _NC_CACHE:
        _NC_CACHE[NT] = build_nc(NT)
    nc = _NC_CACHE[NT]
    maps = make_in_maps(inp, NT, NCORES)
    res = run_bass_kernel_spmd(nc, maps, core_ids=list(range(NCORES)))
    out = np.stack([r["out"] for r in res.results], axis=0)
    return out.reshape(B, S, D).astype(np.float32)
```
